# Optimizing a Trainium2 kernel written in Bass

```python
import jax
import jax.numpy as jnp
from jax import lax
import numpy as np

D_MODEL = 4096
BATCH = 2
SEQ = 8192
DEPTH = 1

GLA_HEADS = 4
GLA_DK = D_MODEL // 16
GLA_DV = D_MODEL // 8
GLA_KEY = GLA_HEADS * GLA_DK
GLA_VAL = GLA_HEADS * GLA_DV
GLA_RANK = 16
GLA_TAU = 16.0
GLA_CHUNK = 64
SGU_WIDTH = D_MODEL // 2
SGU_GROUPS = 8
SGU_GROUP_DIM = SGU_WIDTH // SGU_GROUPS
SGU_CHUNK = 128
N_EXPERTS = 32
TOP_K = 4
D_EXPERT = D_MODEL // 4
SWIGLU_ALPHA = 1.702
SWIGLU_LIMIT = 7.0
MOE_BLOCK = 128
N_ADA = 6
EPS = 1e-5
IN_SIZES = (GLA_KEY, GLA_KEY, GLA_VAL, GLA_VAL, GLA_RANK, SGU_WIDTH, SGU_WIDTH, D_MODEL, D_MODEL)
D_IN = GLA_KEY * 2 + GLA_VAL * 2 + GLA_RANK + SGU_WIDTH * 2 + D_MODEL * 2

kernel_name = 'hybrid_gla_sgu_moe_adaln'


def _rmsnorm(x, g):
    xf = x.astype(jnp.float32)
    inv = lax.rsqrt(jnp.mean(xf * xf, axis=-1, keepdims=True) + EPS)
    return (xf * inv).astype(x.dtype) * g


def _layernorm(x, g, b):
    xf = x.astype(jnp.float32)
    mu = jnp.mean(xf, axis=-1, keepdims=True)
    var = jnp.mean(jnp.square(xf - mu), axis=-1, keepdims=True)
    return ((xf - mu) * lax.rsqrt(var + EPS)).astype(x.dtype) * g + b


def _split(t, sizes):
    outs, start = [], 0
    for s in sizes:
        outs.append(t[..., start:start + s])
        start += s
    return outs


def _gla(q, k, v, log_a):
    b_, s_ = q.shape[0], q.shape[1]
    n = s_ // GLA_CHUNK

    def chunked(t):
        return t.reshape(b_, n, GLA_CHUNK, GLA_HEADS, -1).transpose(1, 0, 3, 2, 4).astype(jnp.float32)

    q, k, v, log_a = chunked(q), chunked(k), chunked(v), chunked(log_a)
    q = q * (GLA_DK ** -0.5)
    cum = jnp.cumsum(log_a, axis=3)
    cum_last = cum[:, :, :, -1:, :]
    q_abs = q * jnp.exp(cum)
    q_rel = q * jnp.exp(cum - cum_last)
    k_rel = k * jnp.exp(cum_last - cum)
    causal = jnp.tril(jnp.ones((GLA_CHUNK, GLA_CHUNK), dtype=bool))
    scores = jnp.einsum('nbhik,nbhjk->nbhij', q_rel, k_rel)
    scores = jnp.where(causal, scores, 0.0)
    o_intra = jnp.einsum('nbhij,nbhjv->nbhiv', scores, v)
    chunk_decay = jnp.exp(cum_last[:, :, :, 0, :])

    def step(state, inp):
        qa, kr, vc, dec = inp
        o = jnp.einsum('bhik,bhkv->bhiv', qa, state)
        state = dec[..., None] * state + jnp.einsum('bhjk,bhjv->bhkv', kr, vc)
        return state, o

    state0 = jnp.zeros((b_, GLA_HEADS, GLA_DK, GLA_DV), jnp.float32)
    _, o_inter = lax.scan(step, state0, (q_abs, k_rel, v, chunk_decay))
    o = o_intra + o_inter
    return o.transpose(1, 0, 3, 2, 4).reshape(b_, s_, GLA_HEADS, GLA_DV)


def _sgu(u, z, ln_g, ln_b, w_spatial, b_spatial):
    b_, s_ = u.shape[0], u.shape[1]
    n = s_ // SGU_CHUNK
    u = jax.nn.gelu(u)
    z = _layernorm(jax.nn.gelu(z), ln_g, ln_b)
    zc = z.reshape(b_, n, SGU_CHUNK, SGU_GROUPS, SGU_GROUP_DIM)
    mask = jnp.tril(jnp.ones((SGU_CHUNK, SGU_CHUNK), dtype=w_spatial.dtype))
    w = w_spatial * mask
    s = jnp.einsum('gts,bnsgc->bntgc', w, zc) + b_spatial.T[None, None, :, :, None]
    return u * s.reshape(b_, s_, SGU_WIDTH)


def _mixer(h, w_in, w_alpha_up, b_alpha, gla_norm_g, sgu_ln_g, sgu_ln_b, w_spatial, b_spatial,
           w_branch_a, w_branch_b, w_out):
    b_, s_ = h.shape[0], h.shape[1]
    proj = h @ w_in
    q, k, v, g, a_lr, u, z, gate_a, gate_b = _split(proj, IN_SIZES)
    log_a = jax.nn.log_sigmoid((a_lr @ w_alpha_up + b_alpha).astype(jnp.float32)) / GLA_TAU
    heads_k = lambda t: t.reshape(b_, s_, GLA_HEADS, GLA_DK)
    o = _gla(heads_k(q), heads_k(k), v.reshape(b_, s_, GLA_HEADS, GLA_DV), heads_k(log_a))
    o = _rmsnorm(o.astype(h.dtype), gla_norm_g).reshape(b_, s_, GLA_VAL)
    y_a = (o * jax.nn.silu(g)) @ w_branch_a
    y_b = _sgu(u, z, sgu_ln_g, sgu_ln_b, w_spatial, b_spatial) @ w_branch_b
    merged = jax.nn.sigmoid(gate_a) * y_a + jax.nn.sigmoid(gate_b) * y_b
    return merged @ w_out


def _moe(h, w_router, b_router, w_exp_gate, b_exp_gate, w_exp_up, b_exp_up, w_exp_down, b_exp_down):
    b_, s_, d = h.shape
    t = b_ * s_
    xt = h.reshape(t, d)
    logits = xt.astype(jnp.float32) @ w_router.astype(jnp.float32) + b_router.astype(jnp.float32)
    top_val, top_idx = lax.top_k(logits, TOP_K)
    top_w = jax.nn.softmax(top_val, axis=-1)
    n_assign = t * TOP_K
    e_flat = top_idx.reshape(n_assign).astype(jnp.int32)
    tok_flat = jnp.repeat(jnp.arange(t, dtype=jnp.int32), TOP_K)
    w_flat = top_w.reshape(n_assign)
    order = jnp.argsort(e_flat)
    e_sorted, tok_sorted, w_sorted = e_flat[order], tok_flat[order], w_flat[order]
    counts = jnp.zeros((N_EXPERTS,), jnp.int32).at[e_flat].add(1)
    starts = jnp.cumsum(counts) - counts
    padded = (counts + MOE_BLOCK - 1) // MOE_BLOCK * MOE_BLOCK
    padded_end = jnp.cumsum(padded)
    padded_start = padded_end - padded
    dest = padded_start[e_sorted] + (jnp.arange(n_assign, dtype=jnp.int32) - starts[e_sorted])
    n_rows = n_assign + N_EXPERTS * MOE_BLOCK
    n_blocks = n_rows // MOE_BLOCK
    row_tok = jnp.zeros((n_rows,), jnp.int32).at[dest].set(tok_sorted)
    row_w = jnp.zeros((n_rows,), jnp.float32).at[dest].set(w_sorted)
    block_start = jnp.arange(n_blocks, dtype=jnp.int32) * MOE_BLOCK
    block_exp = jnp.minimum(jnp.sum(block_start[:, None] >= padded_end[None, :], axis=1), N_EXPERTS - 1)

    def body(y, inp):
        e, tok, wt = inp
        xb = xt[tok]
        x_glu = jnp.minimum(xb @ w_exp_gate[e] + b_exp_gate[e], SWIGLU_LIMIT)
        x_lin = jnp.clip(xb @ w_exp_up[e] + b_exp_up[e], -SWIGLU_LIMIT, SWIGLU_LIMIT)
        act = x_glu * jax.nn.sigmoid(SWIGLU_ALPHA * x_glu) * (x_lin + 1.0)
        out = act @ w_exp_down[e] + b_exp_down[e]
        y = y.at[tok].add(out * wt[:, None].astype(out.dtype))
        return y, None

    y, _ = lax.scan(body, jnp.zeros_like(xt),
                    (block_exp, row_tok.reshape(n_blocks, MOE_BLOCK), row_w.reshape(n_blocks, MOE_BLOCK)))
    return y.reshape(b_, s_, d)


def setup_inputs(seed: int = 0) -> dict:
    key = jax.random.key(seed)
    ks = jax.random.split(key, 32)
    f32 = jnp.float32
    nrm = lambda k, shape, scale: jax.random.normal(k, shape, f32) * scale
    gain = lambda k, shape: 1.0 + 0.02 * jax.random.normal(k, shape, f32)
    L = DEPTH
    return {
        'x': nrm(ks[0], (BATCH, SEQ, D_MODEL), 1.0),
        'c': nrm(ks[1], (BATCH, D_MODEL), 1.0),
        'w_ada': nrm(ks[2], (L, D_MODEL, N_ADA * D_MODEL), 0.2 * D_MODEL ** -0.5),
        'b_ada': nrm(ks[3], (L, N_ADA * D_MODEL), 0.01),
        'norm_mix_g': gain(ks[4], (L, D_MODEL)),
        'w_in': nrm(ks[5], (L, D_MODEL, D_IN), D_MODEL ** -0.5),
        'w_alpha_up': nrm(ks[6], (L, GLA_RANK, GLA_KEY), GLA_RANK ** -0.5),
        'b_alpha': nrm(ks[7], (L, GLA_KEY), 0.1),
        'gla_norm_g': gain(ks[8], (L, GLA_DV)),
        'sgu_ln_g': gain(ks[9], (L, SGU_WIDTH)),
        'sgu_ln_b': nrm(ks[10], (L, SGU_WIDTH), 0.01),
        'w_spatial': nrm(ks[11], (L, SGU_GROUPS, SGU_CHUNK, SGU_CHUNK), SGU_CHUNK ** -0.5),
        'b_spatial': gain(ks[12], (L, SGU_GROUPS, SGU_CHUNK)),
        'w_branch_a': nrm(ks[13], (L, GLA_VAL, D_MODEL), GLA_VAL ** -0.5),
        'w_branch_b': nrm(ks[14], (L, SGU_WIDTH, D_MODEL), SGU_WIDTH ** -0.5),
        'w_out': nrm(ks[15], (L, D_MODEL, D_MODEL), D_MODEL ** -0.5),
        'norm_ffn_g': gain(ks[16], (L, D_MODEL)),
        'w_router': nrm(ks[17], (L, D_MODEL, N_EXPERTS), D_MODEL ** -0.5),
        'b_router': nrm(ks[18], (L, N_EXPERTS), 0.01),
        'w_exp_gate': nrm(ks[19], (L, N_EXPERTS, D_MODEL, D_EXPERT), D_MODEL ** -0.5),
        'b_exp_gate': nrm(ks[20], (L, N_EXPERTS, D_EXPERT), 0.01),
        'w_exp_up': nrm(ks[21], (L, N_EXPERTS, D_MODEL, D_EXPERT), D_MODEL ** -0.5),
        'b_exp_up': nrm(ks[22], (L, N_EXPERTS, D_EXPERT), 0.01),
        'w_exp_down': nrm(ks[23], (L, N_EXPERTS, D_EXPERT, D_MODEL), D_EXPERT ** -0.5),
        'b_exp_down': nrm(ks[24], (L, N_EXPERTS, D_MODEL), 0.01),
        'norm_final_g': gain(ks[25], (D_MODEL,)),
    }


def reference(x, c, w_ada, b_ada, norm_mix_g, w_in, w_alpha_up, b_alpha, gla_norm_g, sgu_ln_g, sgu_ln_b,
              w_spatial, b_spatial, w_branch_a, w_branch_b, w_out, norm_ffn_g, w_router, b_router,
              w_exp_gate, b_exp_gate, w_exp_up, b_exp_up, w_exp_down, b_exp_down, norm_final_g):
    for l in range(DEPTH):
        mod = jax.nn.silu(c) @ w_ada[l] + b_ada[l]
        sh1, sc1, g1, sh2, sc2, g2 = jnp.split(mod, N_ADA, axis=-1)
        h = _rmsnorm(x, norm_mix_g[l]) * (1.0 + sc1[:, None, :]) + sh1[:, None, :]
        x = x + g1[:, None, :] * _mixer(h, w_in[l], w_alpha_up[l], b_alpha[l], gla_norm_g[l], sgu_ln_g[l],
                                        sgu_ln_b[l], w_spatial[l], b_spatial[l], w_branch_a[l],
                                        w_branch_b[l], w_out[l])
        h = _rmsnorm(x, norm_ffn_g[l]) * (1.0 + sc2[:, None, :]) + sh2[:, None, :]
        x = x + g2[:, None, :] * _moe(h, w_router[l], b_router[l], w_exp_gate[l], b_exp_gate[l],
                                      w_exp_up[l], b_exp_up[l], w_exp_down[l], b_exp_down[l])
    return _rmsnorm(x, norm_final_g)
```

```python
import numpy as np
from contextlib import ExitStack
import concourse.bass as bass
import concourse.mybir as mybir
from concourse.bass_utils import run_bass_kernel_spmd

F32 = mybir.dt.float32
BF16 = mybir.dt.bfloat16
U32 = mybir.dt.uint32
AF = mybir.ActivationFunctionType
ALU = mybir.AluOpType

D = 4096
TOK = 2048
TT = 512
NTT = TOK // TT
NPRE = 3 * NTT
KD = D // 128
EPS = 1e-5
OFF_Q, OFF_K, OFF_V, OFF_G, OFF_A, OFF_U, OFF_Z, OFF_GA, OFF_GB = 0, 1024, 2048, 4096, 6144, 6160, 8208, 10256, 14352
NE = 32
CAP = 512
DE = 1024
DEBUG = False
STAGE = 2


class Prog:
    ENG = ["pe", "act", "dve", "pool", "sp"]

    def __init__(self, nc, es, nlanes=20):
        self.nc = nc
        self.eng = {"pe": nc.tensor, "act": nc.scalar, "dve": nc.vector, "pool": nc.gpsimd, "sp": nc.sync}
        self.semh = {e: es.enter_context(nc.semaphore("sem_" + e)) for e in self.ENG}
        self.cnt = {e: 0 for e in self.ENG}
        self.lanes = {}
        for q in ("sp", "pool"):
            self.lanes[q] = [0] * nlanes
            for i in range(nlanes):
                self.semh[("lane", q, i)] = es.enter_context(nc.semaphore(f"ln_{q}_{i}"))
        self.lane_rr = {"sp": 0, "pool": 0}
        self.waited = {e: {} for e in self.ENG}
        self.lastw = {}
        self.readers = {}
        self.npe = 0
        self.marks = []

    def _wait(self, e, tickets):
        for (k, v) in tickets:
            if v <= 0:
                continue
            if self.waited[e].get(k, 0) >= v:
                continue
            self.waited[e][k] = v
            self.eng[e].wait_ge(self.semh[k], v)

    def _deps(self, e, reads, writes):
        need = {}

        def add(t):
            if t is None:
                return
            k, v = t
            if e == "pe" and k == "pe":
                return
            if need.get(k, 0) < v:
                need[k] = v
        for b in reads:
            add(self.lastw.get(b))
        for b in writes:
            add(self.lastw.get(b))
            for k, v in self.readers.get(b, {}).items():
                add((k, v))
        return list(need.items())

    def _commit(self, ticket, reads, writes):
        k, v = ticket
        for b in reads:
            r = self.readers.setdefault(b, {})
            if r.get(k, 0) < v:
                r[k] = v
        for b in writes:
            self.lastw[b] = ticket
            self.readers[b] = {}

    def mark(self, name):
        self.marks.append((name, self.npe))

    def op(self, e, fn, reads=(), writes=(), inc=True):
        if e == "pe":
            self.npe += 1
        self._wait(e, self._deps(e, reads, writes))
        ins = fn(self.eng[e])
        if inc:
            self.cnt[e] += 1
            ins.then_inc(self.semh[e], 1)
            t = (e, self.cnt[e])
        else:
            t = (e, self.cnt[e] + 1)
        self._commit(t, reads, writes)

    def dma(self, q, fn, reads=(), writes=()):
        lanes = self.lanes[q]
        i = self.lane_rr[q]
        self.lane_rr[q] = (i + 1) % len(lanes)
        key = ("lane", q, i)
        deps = self._deps(q, reads, writes)
        deps.append((key, lanes[i]))
        self._wait(q, deps)
        ins = fn(self.eng[q])
        lanes[i] += 16
        ins.then_inc(self.semh[key], 16)
        self._commit((key, lanes[i]), reads, writes)

    def barrier(self, pool=True):
        engs = [e for e in self.ENG if pool or e != "pool"]
        ts = [(e, self.cnt[e]) for e in engs]
        for q in self.lanes:
            if q == "pool" and not pool:
                continue
            for i, v in enumerate(self.lanes[q]):
                ts.append((("lane", q, i), v))
        for e in engs:
            self._wait(e, [t for t in ts if t[0] != e])


_uid = [0]


def build_nc():
    nc = bass.Bass("TRN2", target_bir_lowering=False)

    def din(name, shape, dt=F32):
        return nc.dram_tensor(name, list(shape), dt, kind="ExternalInput").ap()

    def dscr(name, shape, dt):
        return nc.dram_tensor(name, list(shape), dt).ap()

    I = {}
    I["xT"] = din("xT", [D, TOK])
    I["xpT"] = din("xpT", [D, NPRE * TT])
    I["pmask"] = din("pmask", [128, 4])
    I["cT"] = din("cT", [128, KD])
    I["consts"] = din("consts", [128, 7 * 128])
    I["w_ada"] = din("w_ada", [D, 6 * D])
    I["b_adaT"] = din("b_adaT", [128, 6 * KD])
    I["gmixT"] = din("gmixT", [128, KD])
    I["w_in"] = din("w_in", [D, 18448])
    I["w_up_ext"] = din("w_up_ext", [32, 1024])
    I["gla_norm_g"] = din("gla_norm_g", [1, 512])
    I["sgu_ln_g"] = din("sgu_ln_g", [1, 2048])
    I["sgu_ln_b"] = din("sgu_ln_b", [1, 2048])
    I["wsT"] = din("wsT", [128, 8, 128])
    I["b_spatial"] = din("b_spatial", [1, 8 * 128])
    I["w_branch_a"] = din("w_branch_a", [2048, D])
    I["w_branch_b"] = din("w_branch_b", [2048, D])
    I["w_out"] = din("w_out", [D, D])
    I["gffnT"] = din("gffnT", [128, KD])
    I["w_routerT"] = din("w_routerT", [128, KD, NE])
    if STAGE >= 2:
        I["w_exp_gate"] = din("w_exp_gate", [NE, D, DE])
        I["b_gateT"] = din("b_gateT", [128, NE, 8])
        I["w_exp_up"] = din("w_exp_up", [NE, D, DE])
        I["b_upT"] = din("b_upT", [128, NE, 8])
        I["w_exp_down"] = din("w_exp_down", [NE, DE, D])
        I["b_exp_down"] = din("b_exp_down", [NE, D])
        I["norm_final_g"] = din("norm_final_g", [1, D])
        I["norm_ffn_g"] = din("norm_ffn_g", [1, D])
        I["b_router"] = din("b_router", [1, NE])
        I["b_ada_row"] = din("b_ada_row", [1, 6 * D])
    out = nc.dram_tensor("out", [TOK, D], F32, kind="ExternalOutput").ap()
    dbg = None
    if DEBUG:
        dbg = nc.dram_tensor("dbg", [TOK, D], F32, kind="ExternalOutput").ap()

    mod_d = dscr("mod_d", [1, 6 * D], F32)
    o_d = dscr("o_d", [TOK, 2048], F32)
    x1_d = dscr("x1_d", [TOK, D], F32)
    if STAGE >= 2:
        Xd = dscr("Xd", [NE * CAP, D], BF16)
        Yd = dscr("Yd", [NE * CAP, D], F32)

    with ExitStack() as es:
        P = Prog(nc, es)

        def sb(es_, name, shape, dt):
            _uid[0] += 1
            return es_.enter_context(nc.sbuf_tensor(f"{name}_{_uid[0]}", list(shape), dt))

        def ps(es_, name, shape, dt):
            _uid[0] += 1
            return es_.enter_context(nc.psum_tensor(f"{name}_{_uid[0]}", list(shape), dt))

        cst = sb(es, "cst", [128, 7 * 128], F32)
        P.dma("sp", lambda q: q.dma_start(out=cst[:], in_=I["consts"][:, :]), writes=["cst"])
        ident_f = cst[:, 0:128]
        ones_f = cst[:, 128:256]
        triC = cst[:, 256:384]
        triR = cst[:, 384:512]
        maskT = cst[:, 512:640]
        ident_b_t = sb(es, "identb", [128, 128], BF16)
        ones_b_t = sb(es, "onesb", [128, 128], BF16)
        P.op("dve", lambda v: v.tensor_copy(out=ident_b_t[:], in_=ident_f), reads=["cst"], writes=["identb"])
        P.op("dve", lambda v: v.tensor_copy(out=ones_b_t[:], in_=ones_f), reads=["cst"], writes=["onesb"])
        ident_b = ident_b_t[:]
        modT = sb(es, "modT", [128, 6 * KD], F32)
        gm1T = sb(es, "gm1T", [128, KD], F32)
        gm2T = sb(es, "gm2T", [128, KD], F32)
        scT = sb(es, "scT", [128, KD], BF16)
        eS = ExitStack()
        S = sb(eS, "S", [128, 8, 512], F32)
        Sbf = sb(eS, "Sbf", [128, 8, 512], BF16)

        with ExitStack() as e0:
            cT = sb(e0, "cT", [128, KD], F32)
            P.dma("sp", lambda q: q.dma_start(out=cT[:], in_=I["cT"][:, :]), writes=["cT"])
            P.op("act", lambda a: a.activation(out=scT[:], in_=cT[:], func=AF.Silu), reads=["cT"], writes=["scT"])
            NWB = 3
            wbuf = [sb(e0, f"wada{i}", [128, 8, 512], BF16) for i in range(NWB)]
            mps = [ps(e0, f"modps{i}", [128, 512], F32) for i in range(2)]
            row = [sb(e0, f"modrow{i}", [1, 512], F32) for i in range(2)]
            wv = I["w_ada"].rearrange("(k p) c -> p k c", p=128)
            it = 0
            for cb in range(16):
                pb = cb % 2
                for kk in range(4):
                    b = it % NWB
                    it += 1
                    P.dma("pool", lambda q, b=b, kk=kk, cb=cb: q.dma_start(
                        out=wbuf[b][:], in_=wv[:, kk * 8:(kk + 1) * 8, cb * 512:(cb + 1) * 512]),
                        writes=[("wada", b)])
                    for k8 in range(8):
                        k = kk * 8 + k8
                        P.op("pe", lambda pe, b=b, k=k, k8=k8, pb=pb: pe.matmul(
                            mps[pb][0:1, :], lhsT=scT[:, k:k + 1], rhs=wbuf[b][:, k8, :],
                            start=(k == 0), stop=(k == KD - 1)),
                            reads=["scT", ("wada", b)], writes=[("modps", pb)], inc=(k8 == 7))
                P.op("dve", lambda v, pb=pb: v.tensor_copy(out=row[pb][:], in_=mps[pb][0:1, :]),
                     reads=[("modps", pb)], writes=[("modrow", pb)])
                P.dma("sp", lambda q, pb=pb, cb=cb: q.dma_start(
                    out=mod_d[0:1, cb * 512:(cb + 1) * 512], in_=row[pb][:]),
                    reads=[("modrow", pb)], writes=[("mod_d", cb)])
            P.barrier()
        def build_modT():
            with ExitStack() as e0:
                t1 = sb(e0, "t1", [128, 6 * KD], F32)
                t2 = sb(e0, "t2", [128, KD], F32)
                t3 = sb(e0, "t3", [128, KD], F32)
                P.dma("sp", lambda q: q.dma_start(out=t1[:], in_=mod_d.rearrange("o (j p) -> p (o j)", p=128),
                                                  allow_slow_non_contiguous=True), writes=["t1"])
                P.dma("sp", lambda q: q.dma_start(out=modT[:], in_=I["b_adaT"][:, :]), reads=["modT"], writes=["modT"])
                P.dma("sp", lambda q: q.dma_start(out=t2[:], in_=I["gmixT"][:, :]), writes=["t2"])
                P.dma("sp", lambda q: q.dma_start(out=t3[:], in_=I["gffnT"][:, :]), writes=["t3"])
                P.op("dve", lambda v: v.tensor_tensor(out=modT[:], in0=modT[:], in1=t1[:], op=ALU.add),
                     reads=["modT", "t1"], writes=["modT"])
                P.op("dve", lambda v: v.scalar_tensor_tensor(out=gm1T[:], in0=modT[:, KD:2 * KD], scalar=1.0, in1=t2[:],
                                                             op0=ALU.add, op1=ALU.mult),
                     reads=["modT", "t2"], writes=["gm1T"])
                P.op("dve", lambda v: v.scalar_tensor_tensor(out=gm2T[:], in0=modT[:, 4 * KD:5 * KD], scalar=1.0, in1=t3[:],
                                                             op0=ALU.add, op1=ALU.mult),
                     reads=["modT", "t3"], writes=["gm2T"])
                P.barrier()

        build_modT()
        sh1T = modT[:, 0:KD]
        g1T = modT[:, 2 * KD:3 * KD]
        sh2T = modT[:, 3 * KD:4 * KD]

        w_in_v = I["w_in"].rearrange("(k p) c -> p k c", p=128)
        xT_v = I["xT"].rearrange("(k p) t -> p k t", p=128)
        xpT_v = I["xpT"].rearrange("(k p) t -> p k t", p=128)

        def hT_steps(xview, col0, hT, hkeys, xb, sq, rbc, ssp, sskey):
            for k in range(KD):
                b = k % len(xb)
                P.dma("sp", lambda q, b=b, k=k: q.dma_start(out=xb[b][:], in_=xview[:, k, col0:col0 + TT]),
                      writes=[("xb", b)])
                P.op("act", lambda a, b=b, k=k: a.activation(out=sq[k % 2][:], in_=xb[b][:], func=AF.Square),
                     reads=[("xb", b)], writes=[("sq", k % 2)])
                P.op("pe", lambda pe, k=k: pe.matmul(ssp[:], lhsT=ones_f, rhs=sq[k % 2][:],
                                                     start=(k == 0), stop=(k == KD - 1)),
                     reads=["cst", ("sq", k % 2)], writes=[sskey])
                yield
            P.op("dve", lambda v: v.tensor_scalar(out=rbc[:], in0=ssp[:], scalar1=1.0 / D, scalar2=EPS,
                                                  op0=ALU.mult, op1=ALU.add), reads=[sskey], writes=["rbc"])
            P.op("act", lambda a: a.activation(out=rbc[:], in_=rbc[:], func=AF.Sqrt), reads=["rbc"], writes=["rbc"])
            P.op("dve", lambda v: v.reciprocal(out=rbc[:], in_=rbc[:]), reads=["rbc"], writes=["rbc"])
            yield
            for k in range(KD):
                b = k % len(xb)
                P.dma("sp", lambda q, b=b, k=k: q.dma_start(out=xb[b][:], in_=xview[:, k, col0:col0 + TT]),
                      writes=[("xb", b)])
                P.op("dve", lambda v, b=b, k=k: v.tensor_tensor(out=sq[k % 2][:], in0=xb[b][:], in1=rbc[:], op=ALU.mult),
                     reads=[("xb", b), "rbc"], writes=[("sq", k % 2)])
                P.op("act", lambda a, k=k: a.activation(out=hT[:, k, :], in_=sq[k % 2][:], func=AF.Identity,
                                                        scale=gm1T[:, k:k + 1], bias=sh1T[:, k:k + 1]),
                     reads=[("sq", k % 2), "gm1T", "modT"], writes=[hkeys[k]])
                yield

        def step(gen, n):
            if gen is None:
                return
            for _ in range(n):
                if next(gen, "done") == "done":
                    return

        def compute_hT(esx, xview, col0, hT, ssp, sskey):
            with ExitStack() as e1:
                xb = [sb(e1, f"xb{i}", [128, TT], F32) for i in range(3)]
                sq = [sb(e1, f"sq{i}", [128, TT], F32) for i in range(2)]
                rbc = sb(e1, "rbc", [128, TT], F32)
                step(hT_steps(xview, col0, hT, hT_keys, xb, sq, rbc, ssp, sskey), 1000)
                P.barrier(pool=False)

        hT_keys = [("hT", k) for k in range(KD)]

        class WCache:
            def __init__(self, name, nblk, nk):
                self.name = name
                self.ap = dscr(name, [nblk, 128, nk, 128], BF16)
                self.idx = {}
                self.done = set()

            def slot(self, c0):
                if c0 not in self.idx:
                    self.idx[c0] = len(self.idx)
                return self.idx[c0]

        class WStream:
            def __init__(self, esx, name, nbuf, kmax):
                self.name = name
                self.bufs = [sb(esx, f"{name}{i}", [128, kmax, 128], BF16) for i in range(nbuf)]
                self.i = 0

            def load(self, wview, c0, ncols, nk, cache=None):
                b = self.i % len(self.bufs)
                self.i += 1
                t = self.bufs[b]
                key = (self.name, b)
                if cache is not None:
                    ci = cache.slot(c0)
                    ck = (cache.name, ci)
                    if ci in cache.done:
                        P.dma("pool", lambda q: q.dma_start(out=t[:, 0:nk, 0:ncols], in_=cache.ap[ci][:, 0:nk, 0:ncols]),
                              reads=[ck], writes=[key])
                        return t, key
                P.dma("pool", lambda q: q.dma_start(out=t[:, 0:nk, 0:ncols], in_=wview[:, 0:nk, c0:c0 + ncols]),
                      writes=[key])
                if cache is not None:
                    P.dma("sp", lambda q: q.dma_start(out=cache.ap[ci][:, 0:nk, 0:ncols], in_=t[:, 0:nk, 0:ncols]),
                          reads=[key], writes=[ck])
                    cache.done.add(ci)
                return t, key

        wc_in = WCache("wc_in", 145, KD)
        wc_a = WCache("wc_a", 32, 16)
        wc_b = WCache("wc_b", 32, 16)
        wc_o = WCache("wc_o", 32, KD)

        def proj(wt, wkey, ncols, nk, rhsT, rhs_keys, pst, pskey):
            for k in range(nk):
                P.op("pe", lambda pe, k=k: pe.matmul(pst[0:ncols, :], lhsT=wt[:, k, 0:ncols], rhs=rhsT[:, k, :],
                                                     start=(k == 0), stop=(k == nk - 1)),
                     reads=[wkey, rhs_keys[k]], writes=[pskey], inc=(k == nk - 1))

        P.mark('1A')
        P.op("dve", lambda v: v.memset(S[:], 0.0), writes=["S"])
        P.op("dve", lambda v: v.memset(Sbf[:], 0.0), writes=["Sbf"])
        with ExitStack() as eA:
            wup = sb(eA, "wup", [32, 1024], F32)
            pmk = sb(eA, "pmk", [128, 4], F32)
            P.dma("sp", lambda q: q.dma_start(out=pmk[:], in_=I["pmask"][:, :]), writes=["pmk"])
            P.dma("sp", lambda q: q.dma_start(out=wup[:], in_=I["w_up_ext"][:, :]), writes=["wup"])
            hTs = [sb(eA, f"hT{i}", [128, KD, TT], BF16) for i in range(2)]
            hTk = [[("hT", i, k) for k in range(KD)] for i in range(2)]
            xb_ = [sb(eA, f"xbA{i}", [128, TT], F32) for i in range(2)]
            sq_ = [sb(eA, f"sqA{i}", [128, TT], F32) for i in range(2)]
            rbc_ = sb(eA, "rbcA", [128, TT], F32)
            WS = WStream(eA, "wsA", 3, KD)
            a_ext = sb(eA, "a_ext", [32, TT], F32)
            spt = sb(eA, "spt", [128, 4, 1024], F32)
            Ecum = sb(eA, "Ecum", [128, TT], F32)
            Ek = sb(eA, "Ek", [128, TT], F32)
            Eq = sb(eA, "Eq", [128, TT], F32)
            qabs = sb(eA, "qabs", [128, 8, TT], BF16)
            qrel = sb(eA, "qrel", [128, 8, TT], BF16)
            krel = sb(eA, "krel", [128, 8, TT], BF16)
            kreltok = sb(eA, "kreltok", [128, 4, 1024], BF16)
            vtok = sb(eA, "vtok", [128, 4, 2048], BF16)
            vtmp = [sb(eA, f"vtmp{i}", [128, TT], BF16) for i in range(1)]
            dec = sb(eA, "dec", [128, 8, 4], F32)
            sT = [sb(eA, f"sT{i}", [128, 128], BF16) for i in range(2)]
            ost = [sb(eA, f"ost{i}", [128, 512], F32) for i in range(1)]
            pacc = [ps(eA, f"pacc{i}", [128, 512], F32) for i in range(2)]
            pC = ps(eA, "pC", [128, 512], F32)
            pR = ps(eA, "pR", [128, 512], F32)
            pT = [ps(eA, "pT0", [128, 1024], BF16)]
            pSS = ps(eA, "pSS", [128, 512], F32)
            pG = [ps(eA, f"pG{i}", [128, 512], F32) for i in range(2)]
            nacc = [0]

            def next_acc():
                i = nacc[0] % 2
                nacc[0] += 1
                return pacc[i], ("pacc", i)
            ntr = [0]

            def next_tr():
                return pT[0], ("pT", 0)

            def tile_src(it_):
                return (xpT_v, it_ * TT) if it_ < NPRE else (xT_v, (it_ - NPRE) * TT)

            def hT_gen(it_):
                if it_ >= NPRE + NTT:
                    return None
                xv, c0_ = tile_src(it_)
                return hT_steps(xv, c0_, hTs[it_ % 2], hTk[it_ % 2], xb_, sq_, rbc_, pSS, "pSS")

            wada_v = I["w_ada"].rearrange("(k p) c -> p k c", p=128)
            mrow = [sb(eA, f"mrow{i}", [1, 128], F32) for i in range(2)]

            def mod_rest_gen():
                for blk in range(2 * KD, 6 * KD):
                    wt_, wk_ = WS.load(wada_v, blk * 128, 128, KD)
                    for k in range(KD):
                        P.op("pe", lambda pe, k=k, wt_=wt_: pe.matmul(pG[0][0:1, 0:128], lhsT=scT[:, k:k + 1], rhs=wt_[:, k, :],
                                                                      start=(k == 0), stop=(k == KD - 1)),
                             reads=["scT", wk_], writes=[("pG", 0)], inc=(k == KD - 1))
                    mb = blk % 2
                    P.op("dve", lambda v, mb=mb: v.tensor_copy(out=mrow[mb][:], in_=pG[0][0:1, 0:128]),
                         reads=[("pG", 0)], writes=[("mrow", mb)])
                    P.dma("sp", lambda q, mb=mb, blk=blk: q.dma_start(out=mod_d[0:1, blk * 128:(blk + 1) * 128], in_=mrow[mb][:]),
                          reads=[("mrow", mb)], writes=[("mod_d", "r", blk)])
                    yield

            bg2 = mod_rest_gen()
            nproj = [0]

            def step2():
                nproj[0] += 1
                if nproj[0] % 3 == 0:
                    step(bg2, 1)

            step(hT_gen(0), 1000)
            for it in range(NPRE + NTT):
                P.mark(f'1A_t{it}')
                prefix = it < NPRE
                tt = it - NPRE
                hT = hTs[it % 2]
                hT_keys = hTk[it % 2]
                bg = hT_gen(it + 1)
                wt, wk = WS.load(w_in_v, OFF_A, 16, KD, wc_in)
                pa, pk = next_acc()
                proj(wt, wk, 16, KD, hT, hT_keys, pa, pk)
                step(bg, 3)
                step2()
                P.op("dve", lambda v: v.memset(a_ext[:], 1.0), writes=["a_ext"])
                P.op("dve", lambda v, pa=pa: v.tensor_copy(out=a_ext[0:16, :], in_=pa[0:16, :]),
                     reads=[pk, "a_ext"], writes=["a_ext"])
                for sub in range(4):
                    for hf in range(2):
                        P.op("pe", lambda pe, sub=sub, hf=hf: pe.matmul(
                            (pC if hf == 0 else pR)[:], lhsT=a_ext[:, sub * 128:(sub + 1) * 128],
                            rhs=wup[:, hf * 512:(hf + 1) * 512], start=True, stop=True),
                            reads=["a_ext", "wup"], writes=["pC" if hf == 0 else "pR"])
                        P.op("act", lambda a, sub=sub, hf=hf: a.activation(
                            out=spt[:, sub, hf * 512:(hf + 1) * 512], in_=(pC if hf == 0 else pR)[:],
                            func=AF.Exp, scale=-1.0),
                            reads=["pC" if hf == 0 else "pR"], writes=[("spt", sub, hf)])
                        P.op("act", lambda a, sub=sub, hf=hf: a.activation(
                            out=spt[:, sub, hf * 512:(hf + 1) * 512], in_=spt[:, sub, hf * 512:(hf + 1) * 512],
                            func=AF.Ln, bias=1.0, scale=1.0),
                            reads=[("spt", sub, hf)], writes=[("spt", sub, hf)])
                for kc in range(8):
                    hf = kc // 4
                    for sub in range(4):
                        P.op("pe", lambda pe, sub=sub, kc=kc: pe.matmul(
                            pC[:, sub * 128:(sub + 1) * 128], lhsT=spt[:, sub, kc * 128:(kc + 1) * 128], rhs=triC,
                            start=True, stop=True), reads=[("spt", sub, hf), "cst"], writes=["pC"])
                        P.op("pe", lambda pe, sub=sub, kc=kc: pe.matmul(
                            pR[:, sub * 128:(sub + 1) * 128], lhsT=spt[:, sub, kc * 128:(kc + 1) * 128], rhs=triR,
                            start=True, stop=True), reads=[("spt", sub, hf), "cst"], writes=["pR"])
                    P.op("act", lambda a: a.activation(out=Ecum[:], in_=pC[:], func=AF.Exp), reads=["pC"], writes=["Ecum"])
                    P.op("act", lambda a: a.activation(out=Ek[:], in_=pR[:], func=AF.Exp), reads=["pR"], writes=["Ek"])
                    if not prefix:
                        P.op("act", lambda a: a.activation(out=Eq[:], in_=pR[:], func=AF.Exp, scale=-1.0),
                             reads=["pR"], writes=["Eq"])
                    for c in range(4):
                        P.op("dve", lambda v, c=c, kc=kc: v.tensor_copy(
                            out=dec[:, kc, c:c + 1], in_=Ecum[:, c * 128 + 127:c * 128 + 128]),
                            reads=["Ecum"], writes=[("dec", kc)])
                    if not prefix:
                        wt, wk = WS.load(w_in_v, OFF_Q + kc * 128, 128, KD, wc_in)
                        pa, pk = next_acc()
                        proj(wt, wk, 128, KD, hT, hT_keys, pa, pk)
                        step(bg, 3)
                        step2()
                        P.op("dve", lambda v, pa=pa, kc=kc: v.scalar_tensor_tensor(
                            out=qabs[:, kc, :], in0=pa[:], scalar=1.0 / 16.0, in1=Ecum[:], op0=ALU.mult, op1=ALU.mult),
                            reads=[pk, "Ecum"], writes=[("qabs", kc)])
                        P.op("dve", lambda v, pa=pa, kc=kc: v.scalar_tensor_tensor(
                            out=qrel[:, kc, :], in0=pa[:], scalar=1.0 / 16.0, in1=Eq[:], op0=ALU.mult, op1=ALU.mult),
                            reads=[pk, "Eq"], writes=[("qrel", kc)])
                    wt, wk = WS.load(w_in_v, OFF_K + kc * 128, 128, KD, wc_in)
                    pa, pk = next_acc()
                    proj(wt, wk, 128, KD, hT, hT_keys, pa, pk)
                    step(bg, 3)
                    step2()
                    P.op("dve", lambda v, pa=pa, kc=kc: v.tensor_tensor(out=krel[:, kc, :], in0=pa[:], in1=Ek[:], op=ALU.mult),
                         reads=[pk, "Ek"], writes=[("krel", kc)])
                    tp, tk = next_tr()
                    for sub in range(4):
                        P.op("pe", lambda pe, sub=sub, kc=kc, tp=tp: pe.transpose(
                            out=tp[:, sub * 128:(sub + 1) * 128], in_=krel[:, kc, sub * 128:(sub + 1) * 128], identity=ident_b),
                            reads=[("krel", kc), "identb"], writes=[tk], inc=(sub == 3))
                    P.op("act", lambda a, kc=kc, tp=tp: a.activation(
                        out=kreltok[:, :, kc * 128:(kc + 1) * 128],
                        in_=tp[:, 0:512].rearrange("p (s c) -> p s c", s=4), func=AF.Copy),
                        reads=[tk], writes=[("kreltok", kc)])
                for vc in range(16):
                    wt, wk = WS.load(w_in_v, OFF_V + vc * 128, 128, KD, wc_in)
                    pa, pk = next_acc()
                    proj(wt, wk, 128, KD, hT, hT_keys, pa, pk)
                    step(bg, 3)
                    step2()
                    vb = 0
                    P.op("act", lambda a, pa=pa, vb=vb: a.activation(out=vtmp[vb][:], in_=pa[:], func=AF.Copy),
                         reads=[pk], writes=[("vtmp", vb)])
                    tp, tk = next_tr()
                    for sub in range(4):
                        P.op("pe", lambda pe, sub=sub, vb=vb, tp=tp: pe.transpose(
                            out=tp[:, sub * 128:(sub + 1) * 128], in_=vtmp[vb][:, sub * 128:(sub + 1) * 128], identity=ident_b),
                            reads=[("vtmp", vb), "identb"], writes=[tk], inc=(sub == 3))
                    P.op("dve", lambda v, vc=vc, tp=tp: v.tensor_copy(
                        out=vtok[:, :, vc * 128:(vc + 1) * 128], in_=tp[:, 0:512].rearrange("p (s c) -> p s c", s=4)),
                        reads=[tk], writes=[("vtok", vc // 4)])
                for c in range(4):
                    for h in range(4):
                        k0, k1 = 2 * h, 2 * h + 1
                        if not prefix:
                            sp_ = pC if h % 2 == 0 else pR
                            skey = "pC" if h % 2 == 0 else "pR"
                            cs = slice(c * 128, (c + 1) * 128)
                            for i, kc in enumerate((k0, k1)):
                                P.op("pe", lambda pe, kc=kc, i=i, sp_=sp_, cs=cs: pe.matmul(
                                    sp_[:, 0:128], lhsT=krel[:, kc, cs], rhs=qrel[:, kc, cs], start=(i == 0), stop=(i == 1)),
                                    reads=[("krel", kc), ("qrel", kc)], writes=[skey], inc=(i == 1))
                            sb_ = sT[h % 2]
                            P.op("dve", lambda v, sb_=sb_, sp_=sp_: v.tensor_tensor(out=sb_[:], in0=sp_[:, 0:128], in1=maskT, op=ALU.mult),
                                 reads=[skey, "cst"], writes=[("sT", h % 2)])
                            po, pok = next_acc()
                            P.op("pe", lambda pe, po=po, sb_=sb_, c=c, h=h: pe.matmul(
                                po[:], lhsT=sb_[:], rhs=vtok[:, c, h * 512:(h + 1) * 512], start=True, stop=False),
                                reads=[("sT", h % 2), ("vtok", h)], writes=[pok], inc=False)
                            for i, kc in enumerate((k0, k1)):
                                P.op("pe", lambda pe, po=po, kc=kc, i=i, cs=cs: pe.matmul(
                                    po[:], lhsT=qabs[:, kc, cs], rhs=Sbf[:, kc, :], start=False, stop=(i == 1)),
                                    reads=[("qabs", kc), ("Sbf", kc)], writes=[pok], inc=(i == 1))
                            ob = ost[0]
                            P.op("act", lambda a, ob=ob, po=po: a.activation(out=ob[:], in_=po[:], func=AF.Copy),
                                 reads=[pok], writes=[("ost", 0)])
                            r0 = tt * TT + c * 128
                            P.dma("sp", lambda q, ob=ob, r0=r0, h=h: q.dma_start(
                                out=o_d[r0:r0 + 128, h * 512:(h + 1) * 512], in_=ob[:]),
                                reads=[("ost", 0)], writes=[("o_d", tt, c, h)])
                        for i, kc in enumerate((k0, k1)):
                            pg = pG[i]
                            P.op("pe", lambda pe, pg=pg, kc=kc, c=c, h=h: pe.matmul(
                                pg[:], lhsT=kreltok[:, c, kc * 128:(kc + 1) * 128], rhs=vtok[:, c, h * 512:(h + 1) * 512],
                                start=True, stop=True),
                                reads=[("kreltok", kc), ("vtok", h)], writes=[("pG", i)])
                            P.op("dve", lambda v, pg=pg, kc=kc, c=c: v.scalar_tensor_tensor(
                                out=S[:, kc, :], in0=S[:, kc, :], scalar=dec[:, kc, c:c + 1], in1=pg[:],
                                op0=ALU.mult, op1=ALU.add),
                                reads=[("S", kc), ("dec", kc), ("pG", i)], writes=[("S", kc)])
                            P.op("act", lambda a, kc=kc: a.activation(out=Sbf[:, kc, :], in_=S[:, kc, :], func=AF.Copy),
                                 reads=[("S", kc)], writes=[("Sbf", kc)])
                if prefix and it % NTT == NTT - 1:
                    j_ = it // NTT
                    for kc in range(8):
                        P.op("dve", lambda v, kc=kc, j_=j_: v.tensor_scalar(out=S[:, kc, :], in0=S[:, kc, :], scalar1=pmk[:, j_:j_ + 1],
                                                                          scalar2=None, op0=ALU.mult),
                             reads=[("S", kc), "pmk"], writes=[("S", kc)])
                        P.op("act", lambda a, kc=kc: a.activation(out=Sbf[:, kc, :], in_=S[:, kc, :], func=AF.Copy),
                             reads=[("S", kc)], writes=[("Sbf", kc)])
                step(bg, 1000)
            step(bg2, 1000)
            P.barrier()

        P.barrier()
        eS.close()
        build_modT()

        P.mark('1B')
        lgT = sb(es, "lgT", [32, TOK], F32)
        wa_v = I["w_branch_a"].rearrange("(k p) c -> p k c", p=128)
        wb_v = I["w_branch_b"].rearrange("(k p) c -> p k c", p=128)
        wo_v = I["w_out"].rearrange("(k p) c -> p k c", p=128)
        with ExitStack() as eB:
            hT = sb(eB, "hTb", [128, KD, TT], BF16)
            ogT = sb(eB, "ogT", [128, 16, TT], BF16)
            suT = sb(eB, "suT", [128, 16, TT], BF16)
            WS = WStream(eB, "wsB", 4, KD)
            gn_bc = sb(eB, "gn_bc", [128, 512], F32)
            P.dma("sp", lambda q: q.dma_start(out=gn_bc[:], in_=I["gla_norm_g"][0, :].partition_broadcast(128)),
                  writes=["gn_bc"])
            wsT_b = sb(eB, "wsT_b", [128, 8, 128], BF16)
            eW = ExitStack()
            wsT_f = sb(eW, "wsT_f", [128, 8, 128], F32)
            P.dma("sp", lambda q: q.dma_start(out=wsT_f[:], in_=I["wsT"][:, :, :]), writes=["wsT_f"])
            for g in range(8):
                P.op("dve", lambda v, g=g: v.tensor_tensor(out=wsT_b[:, g, :], in0=wsT_f[:, g, :], in1=maskT, op=ALU.mult),
                     reads=["wsT_f", "cst"], writes=["wsT_b"])
            P.barrier()
            eW.close()
            bsp = sb(eB, "bsp", [1, 1024], F32)
            bhi = sb(eB, "bhi", [1, 1024], BF16)
            blo = sb(eB, "blo", [1, 1024], BF16)
            P.dma("sp", lambda q: q.dma_start(out=bsp[:], in_=I["b_spatial"][:, :]), writes=["bsp"])
            P.op("dve", lambda v: v.tensor_copy(out=bhi[:], in_=bsp[:]), reads=["bsp"], writes=["bhi"])
            P.op("dve", lambda v: v.tensor_tensor(out=bsp[:], in0=bsp[:], in1=bhi[:], op=ALU.subtract),
                 reads=["bsp", "bhi"], writes=["bsp"])
            P.op("dve", lambda v: v.tensor_copy(out=blo[:], in_=bsp[:]), reads=["bsp"], writes=["blo"])
            wr = sb(eB, "wr", [128, KD, NE], F32)
            P.dma("sp", lambda q: q.dma_start(out=wr[:], in_=I["w_routerT"][:, :, :]), writes=["wr"])
            for k in range(KD):
                P.op("dve", lambda v, k=k: v.tensor_scalar(out=wr[:, k, :], in0=wr[:, k, :], scalar1=gm2T[:, k:k + 1],
                                                           scalar2=None, op0=ALU.mult), reads=["wr", "gm2T"], writes=["wr"])
            pacc = [ps(eB, f"pb_acc{i}", [128, 512], F32) for i in range(4)]
            pT = [ps(eB, f"pb_T{i}", [128, 1024], BF16) for i in range(2)]
            pX = ps(eB, "pb_X", [128, 512], F32)
            pL = ps(eB, "pb_L", [128, 512], F32)
            nacc = [0]

            def next_acc():
                i = nacc[0] % 4
                nacc[0] += 1
                return pacc[i], ("pb_acc", i)
            ntr = [0]

            def next_tr():
                i = ntr[0] % 2
                ntr[0] += 1
                return pT[i], ("pb_T", i)

            def gelu_from_psum(esx, pa, pk, outap, outkey, tmps, sfx=0):
                xs, t = tmps
                kx, kt = ("g_xs", sfx), ("g_t", sfx)
                P.op("act", lambda a: a.activation(out=xs[:], in_=pa[:], func=AF.Copy), reads=[pk], writes=[kx])
                P.op("dve", lambda v: v.tensor_tensor(out=t[:], in0=xs[:], in1=xs[:], op=ALU.mult), reads=[kx], writes=[kt])
                P.op("dve", lambda v: v.tensor_scalar(out=t[:], in0=t[:], scalar1=0.044715, scalar2=1.0, op0=ALU.mult, op1=ALU.add),
                     reads=[kt], writes=[kt])
                P.op("dve", lambda v: v.tensor_tensor(out=t[:], in0=t[:], in1=xs[:], op=ALU.mult), reads=[kt, kx], writes=[kt])
                P.op("act", lambda a: a.activation(out=t[:], in_=t[:], func=AF.Sigmoid, scale=1.5957691216057308),
                     reads=[kt], writes=[kt])
                P.op("dve", lambda v: v.tensor_tensor(out=outap, in0=xs[:], in1=t[:], op=ALU.mult), reads=[kx, kt], writes=[outkey])

            for tt in range(NTT):
                P.mark(f'1B_t{tt}')
                compute_hT(eB, xT_v, tt * TT, hT, pX, "pb_X")
                with ExitStack() as e2:
                    on = sb(e2, "on", [128, 4, 2048], BF16)
                    ob = [sb(e2, f"ob{i}", [128, 2048], F32) for i in range(2)]
                    ss = sb(e2, "ss", [128, 4], F32)
                    junk = sb(e2, "junk", [128, 512], F32)
                    sg = [sb(e2, f"sg{i}", [128, TT], BF16) for i in range(2)]
                    for sub in range(4):
                        o_ = ob[sub % 2]
                        okey = ("ob", sub % 2)
                        r0 = tt * TT + sub * 128
                        P.dma("sp", lambda q, o_=o_, r0=r0: q.dma_start(out=o_[:], in_=o_d[r0:r0 + 128, :]),
                              reads=[("o_d", tt, sub, h_) for h_ in range(4)], writes=[okey])
                        for h in range(4):
                            hs = slice(h * 512, (h + 1) * 512)
                            P.op("act", lambda a, o_=o_, hs=hs, h=h: a.activation(out=junk[:], in_=o_[:, hs], func=AF.Square,
                                                                                  accum_out=ss[:, h:h + 1]),
                                 reads=[okey], writes=["junk", "ss"])
                        P.op("dve", lambda v: v.tensor_scalar(out=ss[:], in0=ss[:], scalar1=1.0 / 512, scalar2=EPS,
                                                              op0=ALU.mult, op1=ALU.add), reads=["ss"], writes=["ss"])
                        P.op("act", lambda a: a.activation(out=ss[:], in_=ss[:], func=AF.Sqrt), reads=["ss"], writes=["ss"])
                        P.op("dve", lambda v: v.reciprocal(out=ss[:], in_=ss[:]), reads=["ss"], writes=["ss"])
                        for h in range(4):
                            hs = slice(h * 512, (h + 1) * 512)
                            P.op("dve", lambda v, o_=o_, hs=hs, h=h, sub=sub: v.scalar_tensor_tensor(
                                out=on[:, sub, hs], in0=o_[:, hs], scalar=ss[:, h:h + 1], in1=gn_bc[:],
                                op0=ALU.mult, op1=ALU.mult), reads=[okey, "ss", "gn_bc"], writes=[("on", sub)])
                    for vc in range(16):
                        wt, wk = WS.load(w_in_v, OFF_G + vc * 128, 128, KD, wc_in)
                        pa, pk = next_acc()
                        proj(wt, wk, 128, KD, hT, hT_keys, pa, pk)
                        sgb = sg[vc % 2]
                        P.op("act", lambda a, sgb=sgb, pa=pa: a.activation(out=sgb[:], in_=pa[:], func=AF.Silu),
                             reads=[pk], writes=[("sg", vc % 2)])
                        tp, tk = next_tr()
                        for sub in range(4):
                            P.op("pe", lambda pe, sub=sub, vc=vc, tp=tp: pe.transpose(
                                out=tp[:, sub * 128:(sub + 1) * 128], in_=on[:, sub, vc * 128:(vc + 1) * 128], identity=ident_b),
                                reads=[("on", sub), "identb"], writes=[tk], inc=(sub == 3))
                        P.op("dve", lambda v, vc=vc, tp=tp, sgb=sgb: v.tensor_tensor(out=ogT[:, vc, :], in0=tp[:, 0:512], in1=sgb[:], op=ALU.mult),
                             reads=[tk, ("sg", vc % 2)], writes=[("ogT", vc)])
                    P.barrier(pool=False)
                P.mark(f'B3_{tt}')
                with ExitStack() as e3:
                    ztok = sb(e3, "ztok", [128, 4, 2048], BF16)
                    lng = sb(e3, "lng", [128, 2048], F32)
                    lnb = sb(e3, "lnb", [128, 2048], F32)
                    P.dma("sp", lambda q: q.dma_start(out=lng[:], in_=I["sgu_ln_g"][0, :].partition_broadcast(128)), writes=["lng"])
                    P.dma("sp", lambda q: q.dma_start(out=lnb[:], in_=I["sgu_ln_b"][0, :].partition_broadcast(128)), writes=["lnb"])
                    zn = sb(e3, "zn", [128, 4, 2048], BF16)
                    gx = [sb(e3, f"gx{i}", [128, TT], F32) for i in range(2)]
                    gt = [sb(e3, f"gt{i}", [128, TT], F32) for i in range(2)]
                    gz = [sb(e3, f"gz{i}", [128, TT], BF16) for i in range(2)]
                    zf = sb(e3, "zf", [128, 2048], F32)
                    st6 = sb(e3, "st6", [128, 4, 6], F32)
                    mv = sb(e3, "mv", [128, 2], F32)
                    rs = sb(e3, "rs", [128, 2], F32)
                    for zc in range(16):
                        wt, wk = WS.load(w_in_v, OFF_Z + zc * 128, 128, KD, wc_in)
                        pa, pk = next_acc()
                        proj(wt, wk, 128, KD, hT, hT_keys, pa, pk)
                        gzb = gz[zc % 2]
                        gelu_from_psum(e3, pa, pk, gzb[:], ("gz", zc % 2), (gx[zc % 2], gt[zc % 2]), zc % 2)
                        tp, tk = next_tr()
                        for sub in range(4):
                            P.op("pe", lambda pe, sub=sub, gzb=gzb, tp=tp: pe.transpose(
                                out=tp[:, sub * 128:(sub + 1) * 128], in_=gzb[:, sub * 128:(sub + 1) * 128], identity=ident_b),
                                reads=[("gz", zc % 2), "identb"], writes=[tk], inc=(sub == 3))
                        P.op("act", lambda a, zc=zc, tp=tp: a.activation(
                            out=ztok[:, :, zc * 128:(zc + 1) * 128], in_=tp[:, 0:512].rearrange("p (s c) -> p s c", s=4), func=AF.Copy),
                            reads=[tk], writes=["ztok"])
                    for sub in range(4):
                        for i in range(4):
                            P.op("dve", lambda v, sub=sub, i=i: v.bn_stats(out=st6[:, i, :], in_=ztok[:, sub, i * 512:(i + 1) * 512]),
                                 reads=["ztok"], writes=["st6"])
                        P.op("dve", lambda v: v.bn_aggr(out=mv[:], in_=st6[:].rearrange("p a b -> p (a b)")), reads=["st6"], writes=["mv"])
                        P.op("dve", lambda v: v.tensor_scalar(out=rs[:, 0:1], in0=mv[:, 1:2], scalar1=EPS, scalar2=None, op0=ALU.add),
                             reads=["mv"], writes=["rs"])
                        P.op("act", lambda a: a.activation(out=rs[:, 0:1], in_=rs[:, 0:1], func=AF.Sqrt), reads=["rs"], writes=["rs"])
                        P.op("dve", lambda v: v.reciprocal(out=rs[:, 0:1], in_=rs[:, 0:1]), reads=["rs"], writes=["rs"])
                        P.op("dve", lambda v: v.scalar_tensor_tensor(out=rs[:, 1:2], in0=mv[:, 0:1], scalar=-1.0, in1=rs[:, 0:1],
                                                                     op0=ALU.mult, op1=ALU.mult), reads=["mv", "rs"], writes=["rs"])
                        P.op("dve", lambda v, sub=sub: v.tensor_scalar(out=zf[:], in0=ztok[:, sub, :], scalar1=rs[:, 0:1], scalar2=rs[:, 1:2],
                                                                       op0=ALU.mult, op1=ALU.add), reads=["ztok", "rs"], writes=["zf"])
                        P.op("dve", lambda v: v.tensor_tensor(out=zf[:], in0=zf[:], in1=lng[:], op=ALU.mult), reads=["zf", "lng"], writes=["zf"])
                        P.op("dve", lambda v, sub=sub: v.tensor_tensor(out=zn[:, sub, :], in0=zf[:], in1=lnb[:], op=ALU.add),
                             reads=["zf", "lnb"], writes=["zn"])
                    for uc in range(16):
                        g = uc // 2
                        wt, wk = WS.load(w_in_v, OFF_U + uc * 128, 128, KD, wc_in)
                        pa, pk = next_acc()
                        proj(wt, wk, 128, KD, hT, hT_keys, pa, pk)
                        ub = gz[uc % 2]
                        gelu_from_psum(e3, pa, pk, ub[:], ("gz", uc % 2), (gx[uc % 2], gt[uc % 2]), uc % 2)
                        px, pxk = next_acc()
                        for sub in range(4):
                            cs = slice(sub * 128, (sub + 1) * 128)
                            P.op("pe", lambda pe, px=px, sub=sub, uc=uc, g=g, cs=cs: pe.matmul(
                                px[:, cs], lhsT=zn[:, sub, uc * 128:(uc + 1) * 128], rhs=wsT_b[:, g, :], start=True, stop=False),
                                reads=["zn", "wsT_b"], writes=[pxk], inc=False)
                            P.op("pe", lambda pe, px=px, g=g, cs=cs: pe.matmul(
                                px[:, cs], lhsT=ones_b_t[0:1, :], rhs=bhi[0:1, g * 128:(g + 1) * 128], start=False, stop=False),
                                reads=["onesb", "bhi"], writes=[pxk], inc=False)
                            P.op("pe", lambda pe, px=px, g=g, cs=cs: pe.matmul(
                                px[:, cs], lhsT=ones_b_t[0:1, :], rhs=blo[0:1, g * 128:(g + 1) * 128], start=False, stop=True),
                                reads=["onesb", "blo"], writes=[pxk], inc=(sub == 3))
                        P.op("dve", lambda v, px=px, ub=ub, uc=uc: v.tensor_tensor(out=suT[:, uc, :], in0=px[:], in1=ub[:], op=ALU.mult),
                             reads=[pxk, ("gz", uc % 2)], writes=[("suT", uc)])
                    P.barrier(pool=False)
                P.mark(f'B4_{tt}')
                with ExitStack() as e4:
                    mT = sb(e4, "mT", [128, KD, TT], BF16)
                    sa = sb(e4, "sa", [128, TT], F32)
                    sbb = sb(e4, "sbb", [128, TT], F32)
                    t1 = sb(e4, "m_t1", [128, TT], F32)
                    t2 = sb(e4, "m_t2", [128, TT], F32)
                    og_keys = [("ogT", k) for k in range(16)]
                    su_keys = [("suT", k) for k in range(16)]
                    for j in range(KD):
                        wt, wk = WS.load(wa_v, j * 128, 128, 16, wc_a)
                        pA, pAk = next_acc()
                        proj(wt, wk, 128, 16, ogT, og_keys, pA, pAk)
                        wt, wk = WS.load(w_in_v, OFF_GA + j * 128, 128, KD, wc_in)
                        pGA, pGAk = next_acc()
                        proj(wt, wk, 128, KD, hT, hT_keys, pGA, pGAk)
                        P.op("act", lambda a, pGA=pGA: a.activation(out=sa[:], in_=pGA[:], func=AF.Sigmoid), reads=[pGAk], writes=["sa"])
                        P.op("dve", lambda v, pA=pA: v.tensor_tensor(out=t1[:], in0=pA[:], in1=sa[:], op=ALU.mult), reads=[pAk, "sa"], writes=["m_t1"])
                        wt, wk = WS.load(wb_v, j * 128, 128, 16, wc_b)
                        pB, pBk = next_acc()
                        proj(wt, wk, 128, 16, suT, su_keys, pB, pBk)
                        wt, wk = WS.load(w_in_v, OFF_GB + j * 128, 128, KD, wc_in)
                        pGB, pGBk = next_acc()
                        proj(wt, wk, 128, KD, hT, hT_keys, pGB, pGBk)
                        P.op("act", lambda a, pGB=pGB: a.activation(out=sbb[:], in_=pGB[:], func=AF.Sigmoid), reads=[pGBk], writes=["sbb"])
                        P.op("dve", lambda v, pB=pB: v.tensor_tensor(out=t2[:], in0=pB[:], in1=sbb[:], op=ALU.mult), reads=[pBk, "sbb"], writes=["m_t2"])
                        P.op("dve", lambda v, j=j: v.tensor_tensor(out=mT[:, j, :], in0=t1[:], in1=t2[:], op=ALU.add),
                             reads=["m_t1", "m_t2"], writes=[("mT", j)])
                    P.mark(f'B5_{tt}')
                    mT_keys = [("mT", k) for k in range(KD)]
                    xj = [sb(e4, f"xj{i}", [128, TT], F32) for i in range(2)]
                    x1T = [sb(e4, f"x1T{i}", [128, TT], F32) for i in range(2)]
                    xst = [sb(e4, f"xst{i}", [128, 4, 128], F32) for i in range(2)]
                    for j in range(KD):
                        jb = j % 2
                        wt, wk = WS.load(wo_v, j * 128, 128, KD, wc_o)
                        pO, pOk = next_acc()
                        proj(wt, wk, 128, KD, mT, mT_keys, pO, pOk)
                        P.dma("sp", lambda q, j=j, jb=jb: q.dma_start(out=xj[jb][:], in_=xT_v[:, j, tt * TT:(tt + 1) * TT]),
                              writes=[("xj", jb)])
                        P.op("dve", lambda v, pO=pO, j=j, jb=jb: v.scalar_tensor_tensor(
                            out=x1T[jb][:], in0=pO[:], scalar=g1T[:, j:j + 1], in1=xj[jb][:], op0=ALU.mult, op1=ALU.add),
                            reads=[pOk, "modT", ("xj", jb)], writes=[("x1T", jb)])
                        P.op("pe", lambda pe, j=j, jb=jb: pe.matmul(pL[0:32, :], lhsT=wr[:, j, :], rhs=x1T[jb][:],
                                                                    start=(j == 0), stop=(j == KD - 1)),
                             reads=["wr", ("x1T", jb)], writes=["pb_L"])
                        for sub in range(4):
                            P.op("pe", lambda pe, sub=sub, jb=jb: pe.transpose(
                                out=pX[:, sub * 128:(sub + 1) * 128], in_=x1T[jb][:, sub * 128:(sub + 1) * 128], identity=ident_f),
                                reads=[("x1T", jb), "cst"], writes=["pb_X"], inc=(sub == 3))
                        P.op("act", lambda a, jb=jb: a.activation(out=xst[jb][:], in_=pX[:].rearrange("p (s c) -> p s c", s=4), func=AF.Copy),
                             reads=["pb_X"], writes=[("xst", jb)])
                        P.dma("sp", lambda q, j=j, jb=jb, tt=tt: q.dma_start(
                            out=x1_d[tt * TT:(tt + 1) * TT, j * 128:(j + 1) * 128].rearrange("(s p) c -> p s c", p=128),
                            in_=xst[jb][:]), reads=[("xst", jb)], writes=[("x1_d", tt, j)])
                    P.op("act", lambda a, tt=tt: a.activation(out=lgT[:, tt * TT:(tt + 1) * TT], in_=pL[0:32, :], func=AF.Copy),
                         reads=["pb_L"], writes=["lgT"])
                    P.barrier(pool=False)
            P.barrier()


        NSUB = TOK // 128
        NBLK = CAP // 128
        if STAGE >= 2:
          with ExitStack() as eM:
            slots = sb(eM, "slots", [128, NSUB, 4], mybir.dt.int32)
            bnd_reg = nc.gpsimd.to_reg(NE * CAP - 1)
            wk_all = sb(eM, "wk_all", [128, NSUB, 4], F32)
            P.mark('2a')
            with ExitStack() as e2:
                gm2b = sb(e2, "gm2b", [128, D], F32)
                sh2b = sb(e2, "sh2b", [128, D], F32)
                tA = sb(e2, "tA", [128, D], F32)
                tB = sb(e2, "tB", [128, D], F32)
                bc = lambda ap: ap.partition_broadcast(128)
                P.dma("sp", lambda q: q.dma_start(out=tA[:], in_=bc(mod_d[0, 4 * D:5 * D])), writes=["tA"])
                P.dma("sp", lambda q: q.dma_start(out=tB[:], in_=bc(I["b_ada_row"][0, 4 * D:5 * D])), writes=["tB"])
                P.op("dve", lambda v: v.tensor_tensor(out=tA[:], in0=tA[:], in1=tB[:], op=ALU.add), reads=["tA", "tB"], writes=["tA"])
                P.dma("sp", lambda q: q.dma_start(out=tB[:], in_=bc(I["norm_ffn_g"][0, :])), reads=["tB"], writes=["tB"])
                P.op("dve", lambda v: v.scalar_tensor_tensor(out=gm2b[:], in0=tA[:], scalar=1.0, in1=tB[:], op0=ALU.add, op1=ALU.mult),
                     reads=["tA", "tB"], writes=["gm2b"])
                P.dma("sp", lambda q: q.dma_start(out=tA[:], in_=bc(mod_d[0, 3 * D:4 * D])), reads=["tA"], writes=["tA"])
                P.dma("sp", lambda q: q.dma_start(out=tB[:], in_=bc(I["b_ada_row"][0, 3 * D:4 * D])), reads=["tB"], writes=["tB"])
                P.op("dve", lambda v: v.tensor_tensor(out=sh2b[:], in0=tA[:], in1=tB[:], op=ALU.add), reads=["tA", "tB"], writes=["sh2b"])
                wr2 = sb(e2, "wr2", [128, KD, NE], F32)
                brow = sb(e2, "brow", [1, NE], F32)
                crow = sb(e2, "crow", [1, NE], F32)
                constb = sb(e2, "constb", [128, NE], F32)
                ecap = sb(e2, "ecap", [128, NE], F32)
                macc = sb(e2, "macc", [128, NE], F32)
                pq = ps(e2, "pq", [128, 512], F32)
                pr = ps(e2, "pr", [128, 512], F32)
                P.dma("sp", lambda q: q.dma_start(out=wr2[:], in_=I["w_routerT"][:, :, :]), writes=["wr2"])
                P.dma("sp", lambda q: q.dma_start(out=brow[:], in_=I["b_router"][:, :]), writes=["brow"])
                for k in range(KD):
                    P.op("pe", lambda pe, k=k: pe.matmul(pq[0:1, 0:NE], lhsT=sh2T[:, k:k + 1], rhs=wr2[:, k, :],
                                                         start=(k == 0), stop=(k == KD - 1)), reads=["modT", "wr2"], writes=["pq"], inc=(k == KD - 1))
                P.op("dve", lambda v: v.tensor_tensor(out=crow[:], in0=pq[0:1, 0:NE], in1=brow[:], op=ALU.add), reads=["pq", "brow"], writes=["crow"])
                P.op("pe", lambda pe: pe.matmul(pq[:, 0:NE], lhsT=ones_f[0:1, :], rhs=crow[:], start=True, stop=True),
                     reads=["cst", "crow"], writes=["pq"])
                P.op("dve", lambda v: v.tensor_copy(out=constb[:], in_=pq[:, 0:NE]), reads=["pq"], writes=["constb"])
                P.op("dve", lambda v: v.tensor_scalar(out=ecap[:], in0=cst[:, 768:800], scalar1=float(CAP), scalar2=None, op0=ALU.mult),
                     reads=["cst"], writes=["ecap"])
                P.op("dve", lambda v: v.memset(macc[:], 0.0), writes=["macc"])
                x1t = [sb(e2, f"x1t{i}", [128, D], F32) for i in range(2)]
                h2t = [sb(e2, f"h2t{i}", [128, D], BF16) for i in range(2)]
                sm = sb(e2, "sm", [128, 8], F32)
                lg = sb(e2, "lg", [128, NE], F32)
                m8 = sb(e2, "m8", [128, 8], F32)
                msk = sb(e2, "msk", [128, NE], F32)
                pe_ = sb(e2, "pe_", [128, NE], F32)
                wts = sb(e2, "wts", [128, NE], F32)
                slotd = sb(e2, "slotd", [128, NE], F32)
                oh = sb(e2, "oh", [128, NE], F32)
                jk = sb(e2, "jk", [128, NE], F32)
                slf = sb(e2, "slf", [128, 4], F32)
                for s_ in range(NSUB):
                    b = s_ % 2
                    xk, hk = ("x1t", b), ("h2t", b)
                    P.dma("sp", lambda q, b=b, s_=s_: q.dma_start(out=x1t[b][:], in_=x1_d[s_ * 128:(s_ + 1) * 128, :]), writes=[xk])
                    P.op("act", lambda a, b=b: a.activation(out=tA[:], in_=x1t[b][:], func=AF.Square, accum_out=sm[:, 0:1]),
                         reads=[xk], writes=["tA", "sm"])
                    P.op("dve", lambda v: v.tensor_scalar(out=sm[:, 1:2], in0=sm[:, 0:1], scalar1=1.0 / D, scalar2=EPS, op0=ALU.mult, op1=ALU.add),
                         reads=["sm"], writes=["sm"])
                    P.op("act", lambda a: a.activation(out=sm[:, 1:2], in_=sm[:, 1:2], func=AF.Sqrt), reads=["sm"], writes=["sm"])
                    P.op("dve", lambda v: v.reciprocal(out=sm[:, 2:3], in_=sm[:, 1:2]), reads=["sm"], writes=["sm"])
                    P.op("dve", lambda v, b=b: v.scalar_tensor_tensor(out=tB[:], in0=x1t[b][:], scalar=sm[:, 2:3], in1=gm2b[:],
                                                                      op0=ALU.mult, op1=ALU.mult), reads=[xk, "sm", "gm2b"], writes=["tB"])
                    P.op("dve", lambda v, b=b: v.tensor_tensor(out=h2t[b][:], in0=tB[:], in1=sh2b[:], op=ALU.add),
                         reads=["tB", "sh2b"], writes=[hk])
                    P.op("pe", lambda pe, s_=s_: pe.transpose(out=pr[:, 0:NE], in_=lgT[0:32, s_ * 128:(s_ + 1) * 128], identity=ident_f[0:32, 0:32]),
                         reads=["lgT", "cst"], writes=["pr"])
                    P.op("dve", lambda v: v.scalar_tensor_tensor(out=lg[:], in0=pr[:, 0:NE], scalar=sm[:, 2:3], in1=constb[:],
                                                                 op0=ALU.mult, op1=ALU.add), reads=["pr", "sm", "constb"], writes=["lg"])
                    P.op("dve", lambda v: v.max(out=m8[:], in_=lg[:]), reads=["lg"], writes=["m8"])
                    P.op("dve", lambda v: v.tensor_scalar(out=msk[:], in0=lg[:], scalar1=m8[:, 3:4], scalar2=None, op0=ALU.is_ge),
                         reads=["lg", "m8"], writes=["msk"])
                    P.op("dve", lambda v: v.tensor_scalar(out=sm[:, 3:4], in0=m8[:, 0:1], scalar1=-1.0, scalar2=None, op0=ALU.mult),
                         reads=["m8"], writes=["sm"])
                    P.op("act", lambda a: a.activation(out=pe_[:], in_=lg[:], func=AF.Exp, bias=sm[:, 3:4], scale=1.0),
                         reads=["lg", "sm"], writes=["pe_"])
                    P.op("dve", lambda v: v.tensor_tensor(out=wts[:], in0=pe_[:], in1=msk[:], op=ALU.mult),
                         reads=["pe_", "msk"], writes=["wts"])
                    P.op("dve", lambda v: v.tensor_reduce(out=sm[:, 4:5], in_=wts[:], axis=mybir.AxisListType.X, op=ALU.add),
                         reads=["wts"], writes=["sm"])
                    P.op("dve", lambda v: v.reciprocal(out=sm[:, 5:6], in_=sm[:, 4:5]), reads=["sm"], writes=["sm"])
                    P.op("dve", lambda v: v.tensor_scalar(out=wts[:], in0=wts[:], scalar1=sm[:, 5:6], scalar2=None, op0=ALU.mult),
                         reads=["wts", "sm"], writes=["wts"])
                    P.op("pe", lambda pe: pe.matmul(pq[:, 0:NE], lhsT=cst[:, 640:768], rhs=msk[:], start=True, stop=False),
                         reads=["cst", "msk"], writes=["pq"], inc=False)
                    P.op("pe", lambda pe: pe.matmul(pq[:, 0:NE], lhsT=ones_f, rhs=macc[:], start=False, stop=True),
                         reads=["cst", "macc"], writes=["pq"])
                    P.op("dve", lambda v: v.tensor_tensor(out=macc[:], in0=macc[:], in1=msk[:], op=ALU.add), reads=["macc", "msk"], writes=["macc"])
                    P.op("dve", lambda v: v.tensor_scalar(out=jk[:], in0=pq[:, 0:NE], scalar1=float(CAP), scalar2=None, op0=ALU.is_lt),
                         reads=["pq"], writes=["jk"])
                    P.op("dve", lambda v: v.tensor_tensor(out=jk[:], in0=jk[:], in1=msk[:], op=ALU.mult), reads=["jk", "msk"], writes=["jk"])
                    P.op("dve", lambda v: v.tensor_tensor(out=slotd[:], in0=pq[:, 0:NE], in1=ecap[:], op=ALU.add), reads=["pq", "ecap"], writes=["slotd"])
                    P.op("dve", lambda v: v.scalar_tensor_tensor(out=slotd[:], in0=slotd[:], scalar=-float(4 * NE * CAP), in1=jk[:],
                                                                 op0=ALU.add, op1=ALU.mult), reads=["slotd", "jk"], writes=["slotd"])
                    P.op("dve", lambda v: v.tensor_scalar(out=slotd[:], in0=slotd[:], scalar1=float(4 * NE * CAP), scalar2=None, op0=ALU.add),
                         reads=["slotd"], writes=["slotd"])
                    for k in range(4):
                        P.op("dve", lambda v, k=k: v.tensor_scalar(out=oh[:], in0=lg[:], scalar1=m8[:, k:k + 1], scalar2=None, op0=ALU.is_equal),
                             reads=["lg", "m8"], writes=["oh"])
                        P.op("dve", lambda v: v.tensor_tensor(out=jk[:], in0=oh[:], in1=slotd[:], op=ALU.mult),
                             reads=["oh", "slotd"], writes=["jk"])
                        P.op("dve", lambda v, k=k: v.tensor_reduce(out=slf[:, k:k + 1], in_=jk[:], axis=mybir.AxisListType.X, op=ALU.add),
                             reads=["jk"], writes=["slf"])
                        P.op("dve", lambda v: v.tensor_tensor(out=jk[:], in0=oh[:], in1=wts[:], op=ALU.mult),
                             reads=["oh", "wts"], writes=["jk"])
                        P.op("dve", lambda v, k=k, s_=s_: v.tensor_reduce(out=wk_all[:, s_, k:k + 1], in_=jk[:], axis=mybir.AxisListType.X, op=ALU.add),
                             reads=["jk"], writes=["wk_all"])
                    P.op("dve", lambda v, s_=s_: v.tensor_copy(out=slots[:, s_, :], in_=slf[:]), reads=["slf"], writes=["slots"])
                    for k in range(4):
                        P.dma("pool", lambda q, b=b, s_=s_, k=k: q.indirect_dma_start(
                            out=Xd[:, :], out_offset=bass.IndirectOffsetOnAxis(ap=slots[:, s_, k:k + 1].bitcast(U32), axis=0),
                            in_=h2t[b][:], in_offset=None, bounds_check=bnd_reg, oob_is_err=False),
                            reads=[hk, "slots"], writes=[("Xd", s_, k)])
                P.barrier()
            P.mark('2c')
            with ExitStack() as e3:
                XeTs = [sb(e3, f"XeT{i}", [128, KD, CAP], BF16) for i in range(2)]
                XeTk = [[("XeT", i, k) for k in range(KD)] for i in range(2)]
                xtok = [sb(e3, f"xtok{i}", [128, D], BF16) for i in range(2)]
                WG = WStream(e3, "wsG", 4, KD)
                wdb = [sb(e3, f"wdb{i}", [128, 8, 512], BF16) for i in range(2)]
                actT = sb(e3, "actT", [128, 8, CAP], BF16)
                bgT = sb(e3, "bgT", [128, NE, 8], F32)
                buT = sb(e3, "buT", [128, NE, 8], F32)
                bdf = sb(e3, "bdf", [1, D], F32)
                bdb = sb(e3, "bdb", [1, D], BF16)
                xg = sb(e3, "xg", [128, CAP], F32)
                sgm = sb(e3, "sgm", [128, CAP], F32)
                xl = sb(e3, "xl", [128, CAP], F32)
                yst = [sb(e3, f"yst{i}", [128, 512], F32) for i in range(3)]
                P.dma("sp", lambda q: q.dma_start(out=bgT[:], in_=I["b_gateT"][:, :, :]), writes=["bgT"])
                P.dma("sp", lambda q: q.dma_start(out=buT[:], in_=I["b_upT"][:, :, :]), writes=["buT"])
                pTe = [ps(e3, f"pTe{i}", [128, 1024], BF16) for i in range(2)]
                pg_ = [ps(e3, f"pg{i}", [128, 512], F32) for i in range(2)]
                pu_ = [ps(e3, f"pu{i}", [128, 512], F32) for i in range(2)]
                po_ = [ps(e3, f"po{i}", [128, 512], F32) for i in range(2)]
                nx = [0]
                ny = 0
                ndw = 0

                def xet_gen(e_):
                    XeT = XeTs[e_ % 2]
                    for blk in range(NBLK):
                        xb_ = xtok[nx[0] % 2]
                        xbk = ("xtok", nx[0] % 2)
                        nx[0] += 1
                        r0 = e_ * CAP + blk * 128
                        P.dma("sp", lambda q, xb_=xb_, r0=r0: q.dma_start(out=xb_[:], in_=Xd[r0:r0 + 128, :]), writes=[xbk])
                        for k4 in range(8):
                            tp = pTe[k4 % 2]
                            tk = ("pTe", k4 % 2)
                            for kk in range(4):
                                k = k4 * 4 + kk
                                P.op("pe", lambda pe, tp=tp, kk=kk, k=k, xb_=xb_: pe.transpose(
                                    out=tp[:, kk * 128:(kk + 1) * 128], in_=xb_[:, k * 128:(k + 1) * 128], identity=ident_b),
                                    reads=[xbk, "identb"], writes=[tk], inc=(kk == 3))
                            wkeys = [("XeT", e_ % 2, k4 * 4 + kk) for kk in range(4)]
                            if k4 % 2 == 0:
                                P.op("act", lambda a, tp=tp, k4=k4, blk=blk, XeT=XeT: a.activation(
                                    out=XeT[:, k4 * 4:(k4 + 1) * 4, blk * 128:(blk + 1) * 128],
                                    in_=tp[:, 0:512].rearrange("p (s c) -> p s c", s=4), func=AF.Copy), reads=[tk], writes=wkeys)
                            else:
                                P.op("dve", lambda v, tp=tp, k4=k4, blk=blk, XeT=XeT: v.tensor_copy(
                                    out=XeT[:, k4 * 4:(k4 + 1) * 4, blk * 128:(blk + 1) * 128],
                                    in_=tp[:, 0:512].rearrange("p (s c) -> p s c", s=4)), reads=[tk], writes=wkeys)
                            yield

                step(xet_gen(0), 1000)
                for e in range(NE):
                    XeT = XeTs[e % 2]
                    XeT_keys = XeTk[e % 2]
                    bg = xet_gen(e + 1) if e + 1 < NE else None
                    wg_v = I["w_exp_gate"][e].rearrange("(k p) c -> p k c", p=128)
                    wu_v = I["w_exp_up"][e].rearrange("(k p) c -> p k c", p=128)
                    wd_v = I["w_exp_down"][e].rearrange("(k p) c -> p k c", p=128)
                    P.dma("sp", lambda q, e=e: q.dma_start(out=bdf[:], in_=I["b_exp_down"][e:e + 1, :]), writes=["bdf"])
                    P.op("dve", lambda v: v.tensor_copy(out=bdb[:], in_=bdf[:]), reads=["bdf"], writes=["bdb"])
                    for fc in range(8):
                        pg, pgk = pg_[fc % 2], ("pg", fc % 2)
                        pu, puk = pu_[fc % 2], ("pu", fc % 2)
                        wt, wk = WG.load(wg_v, fc * 128, 128, KD)
                        proj(wt, wk, 128, KD, XeT, XeT_keys, pg, pgk)
                        wt, wk = WG.load(wu_v, fc * 128, 128, KD)
                        proj(wt, wk, 128, KD, XeT, XeT_keys, pu, puk)
                        step(bg, 4)
                        P.op("dve", lambda v, pg=pg, e=e, fc=fc: v.tensor_scalar(out=xg[:], in0=pg[:], scalar1=bgT[:, e, fc:fc + 1], scalar2=7.0,
                                                                                 op0=ALU.add, op1=ALU.min), reads=[pgk, "bgT"], writes=["xg"])
                        P.op("act", lambda a: a.activation(out=sgm[:], in_=xg[:], func=AF.Sigmoid, scale=1.702), reads=["xg"], writes=["sgm"])
                        P.op("dve", lambda v, pu=pu, e=e, fc=fc: v.tensor_scalar(out=xl[:], in0=pu[:], scalar1=buT[:, e, fc:fc + 1], scalar2=7.0,
                                                                                 op0=ALU.add, op1=ALU.min), reads=[puk, "buT"], writes=["xl"])
                        P.op("dve", lambda v: v.tensor_scalar(out=xl[:], in0=xl[:], scalar1=-7.0, scalar2=1.0, op0=ALU.max, op1=ALU.add),
                             reads=["xl"], writes=["xl"])
                        P.op("dve", lambda v: v.tensor_tensor(out=xg[:], in0=xg[:], in1=sgm[:], op=ALU.mult), reads=["xg", "sgm"], writes=["xg"])
                        P.op("dve", lambda v, fc=fc: v.tensor_tensor(out=actT[:, fc, :], in0=xg[:], in1=xl[:], op=ALU.mult),
                             reads=["xg", "xl"], writes=[("actT", fc)])
                    step(bg, 1000)
                    for db in range(8):
                        wd = wdb[ndw % 2]
                        wdk = ("wdb", ndw % 2)
                        ndw += 1
                        P.dma("pool", lambda q, wd=wd, db=db, wd_v=wd_v: q.dma_start(out=wd[:], in_=wd_v[:, :, db * 512:(db + 1) * 512]), writes=[wdk])
                        for blk in range(NBLK):
                            po, pok = po_[blk % 2], ("po", blk % 2)
                            for fk in range(8):
                                P.op("pe", lambda pe, po=po, fk=fk, blk=blk, wd=wd: pe.matmul(
                                    po[:], lhsT=actT[:, fk, blk * 128:(blk + 1) * 128], rhs=wd[:, fk, :], start=(fk == 0), stop=False),
                                    reads=[("actT", fk), wdk], writes=[pok], inc=False)
                            P.op("pe", lambda pe, po=po, db=db: pe.matmul(po[:], lhsT=ones_b_t[0:1, :], rhs=bdb[0:1, db * 512:(db + 1) * 512],
                                                                          start=False, stop=True), reads=["onesb", "bdb"], writes=[pok])
                            yb = yst[ny % 3]
                            ybk = ("yst", ny % 3)
                            if ny % 2 == 0:
                                P.op("act", lambda a, yb=yb, po=po: a.activation(out=yb[:], in_=po[:], func=AF.Copy), reads=[pok], writes=[ybk])
                            else:
                                P.op("dve", lambda v, yb=yb, po=po: v.tensor_copy(out=yb[:], in_=po[:]), reads=[pok], writes=[ybk])
                            ny += 1
                            r0 = e * CAP + blk * 128
                            P.dma("sp", lambda q, yb=yb, r0=r0, db=db: q.dma_start(out=Yd[r0:r0 + 128, db * 512:(db + 1) * 512], in_=yb[:]),
                                  reads=[ybk], writes=[("Yd", e, blk, db)])
                P.barrier()
            P.mark('2d')
            with ExitStack() as e4:
                g2b = sb(e4, "g2b", [128, D], F32)
                gfb = sb(e4, "gfb", [128, D], F32)
                tA = sb(e4, "tA2", [128, D], F32)
                bc = lambda ap: ap.partition_broadcast(128)
                P.dma("sp", lambda q: q.dma_start(out=g2b[:], in_=bc(mod_d[0, 5 * D:6 * D])), writes=["g2b"])
                P.dma("sp", lambda q: q.dma_start(out=tA[:], in_=bc(I["b_ada_row"][0, 5 * D:6 * D])), writes=["tA2"])
                P.op("dve", lambda v: v.tensor_tensor(out=g2b[:], in0=g2b[:], in1=tA[:], op=ALU.add), reads=["g2b", "tA2"], writes=["g2b"])
                P.dma("sp", lambda q: q.dma_start(out=gfb[:], in_=bc(I["norm_final_g"][0, :])), writes=["gfb"])
                x1t = [sb(e4, f"x1u{i}", [128, D], F32) for i in range(2)]
                rk = [sb(e4, f"rk{i}", [128, D], F32) for i in range(3)]
                acc = sb(e4, "acc", [128, D], F32)
                sm = sb(e4, "sm2", [128, 4], F32)
                nr = 0
                for s_ in range(NSUB):
                    b = s_ % 2
                    xk = ("x1u", b)
                    P.dma("sp", lambda q, b=b, s_=s_: q.dma_start(out=x1t[b][:], in_=x1_d[s_ * 128:(s_ + 1) * 128, :]), writes=[xk])
                    for k in range(4):
                        r = rk[nr % 3]
                        rkk = ("rk", nr % 3)
                        nr += 1
                        P.dma("pool", lambda q, r=r, s_=s_, k=k: q.indirect_dma_start(
                            out=r[:], out_offset=None, in_=Yd[:, :],
                            in_offset=bass.IndirectOffsetOnAxis(ap=slots[:, s_, k:k + 1].bitcast(U32), axis=0),
                            bounds_check=bnd_reg, oob_is_err=False), reads=["slots"], writes=[rkk])
                        if k == 0:
                            P.op("dve", lambda v, r=r, s_=s_, k=k: v.tensor_scalar(out=acc[:], in0=r[:], scalar1=wk_all[:, s_, k:k + 1], scalar2=None,
                                                                                   op0=ALU.mult), reads=[rkk, "wk_all"], writes=["acc"])
                        else:
                            P.op("dve", lambda v, r=r, s_=s_, k=k: v.scalar_tensor_tensor(out=acc[:], in0=r[:], scalar=wk_all[:, s_, k:k + 1], in1=acc[:],
                                                                                          op0=ALU.mult, op1=ALU.add), reads=[rkk, "wk_all", "acc"], writes=["acc"])
                    P.op("dve", lambda v: v.tensor_tensor(out=acc[:], in0=acc[:], in1=g2b[:], op=ALU.mult), reads=["acc", "g2b"], writes=["acc"])
                    P.op("dve", lambda v, b=b: v.tensor_tensor(out=acc[:], in0=acc[:], in1=x1t[b][:], op=ALU.add), reads=["acc", xk], writes=["acc"])
                    P.op("act", lambda a: a.activation(out=tA[:], in_=acc[:], func=AF.Square, accum_out=sm[:, 0:1]), reads=["acc"], writes=["tA2", "sm2"])
                    P.op("dve", lambda v: v.tensor_scalar(out=sm[:, 1:2], in0=sm[:, 0:1], scalar1=1.0 / D, scalar2=EPS, op0=ALU.mult, op1=ALU.add),
                         reads=["sm2"], writes=["sm2"])
                    P.op("act", lambda a: a.activation(out=sm[:, 1:2], in_=sm[:, 1:2], func=AF.Sqrt), reads=["sm2"], writes=["sm2"])
                    P.op("dve", lambda v: v.reciprocal(out=sm[:, 2:3], in_=sm[:, 1:2]), reads=["sm2"], writes=["sm2"])
                    P.op("dve", lambda v, b=b: v.scalar_tensor_tensor(out=x1t[b][:], in0=acc[:], scalar=sm[:, 2:3], in1=gfb[:], op0=ALU.mult, op1=ALU.mult),
                         reads=["acc", "sm2", "gfb"], writes=[xk])
                    P.dma("sp", lambda q, b=b, s_=s_: q.dma_start(out=out[s_ * 128:(s_ + 1) * 128, :], in_=x1t[b][:]), reads=[xk], writes=[("out", s_)])
                P.barrier()

        if STAGE < 2:
         with ExitStack() as eZ:
             tb = [sb(eZ, f"tb{i}", [128, D], F32) for i in range(2)]
             for s_ in range(TOK // 128):
                 b = s_ % 2
                 P.dma("sp", lambda q, b=b, s_=s_: q.dma_start(out=tb[b][:], in_=x1_d[s_ * 128:(s_ + 1) * 128, :]),
                       writes=[("tb", b)])
                 P.dma("sp", lambda q, b=b, s_=s_: q.dma_start(out=out[s_ * 128:(s_ + 1) * 128, :], in_=tb[b][:]),
                       reads=[("tb", b)], writes=[("out", s_)])
             P.barrier()
    build_nc.marks = P.marks
    return nc


def make_consts():
    c = np.zeros((128, 7 * 128), np.float32)
    j = np.arange(128)[:, None]
    i = np.arange(128)[None, :]
    c[:, 0:128] = np.eye(128, dtype=np.float32)
    c[:, 128:256] = 1.0
    c[:, 256:384] = np.where(j <= i, -1.0 / 16.0, 0.0)
    c[:, 384:512] = np.where(j > i, -1.0 / 16.0, 0.0)
    c[:, 512:640] = np.where(j <= i, 1.0, 0.0)
    c[:, 640:768] = np.where(j < i, 1.0, 0.0)
    c[:, 768:800] = np.arange(32)[None, :]
    return c


def fm(v):
    v = np.asarray(v)
    return np.ascontiguousarray(v.reshape(-1, 128).T)


def prep_inputs(x, c, w_ada, b_ada, norm_mix_g, w_in, w_alpha_up, b_alpha, gla_norm_g, sgu_ln_g, sgu_ln_b,
                w_spatial, b_spatial, w_branch_a, w_branch_b, w_out, norm_ffn_g, w_router, b_router,
                w_exp_gate, b_exp_gate, w_exp_up, b_exp_up, w_exp_down, b_exp_down, norm_final_g):
    f = lambda a: np.ascontiguousarray(np.asarray(a, dtype=np.float32))
    shared = {}
    shared["consts"] = make_consts()
    shared["w_ada"] = f(w_ada[0])
    shared["b_adaT"] = fm(f(b_ada[0]))
    shared["b_ada_row"] = f(b_ada[0]).reshape(1, -1)
    shared["gmixT"] = fm(f(norm_mix_g[0]))
    shared["w_in"] = f(w_in[0])
    wup = np.zeros((32, 1024), np.float32)
    wup[0:16] = f(w_alpha_up[0])
    wup[16] = f(b_alpha[0])
    shared["w_up_ext"] = wup
    shared["gla_norm_g"] = f(gla_norm_g[0]).reshape(1, -1)
    shared["sgu_ln_g"] = f(sgu_ln_g[0]).reshape(1, -1)
    shared["sgu_ln_b"] = f(sgu_ln_b[0]).reshape(1, -1)
    shared["wsT"] = np.ascontiguousarray(f(w_spatial[0]).transpose(2, 0, 1))
    shared["b_spatial"] = f(b_spatial[0]).reshape(1, -1)
    shared["w_branch_a"] = f(w_branch_a[0])
    shared["w_branch_b"] = f(w_branch_b[0])
    shared["w_out"] = f(w_out[0])
    shared["gffnT"] = fm(f(norm_ffn_g[0]))
    shared["norm_ffn_g"] = f(norm_ffn_g[0]).reshape(1, -1)
    shared["w_routerT"] = np.ascontiguousarray(f(w_router[0]).reshape(KD, 128, NE).transpose(1, 0, 2))
    shared["b_router"] = f(b_router[0]).reshape(1, -1)
    shared["w_exp_gate"] = f(w_exp_gate[0])
    shared["b_gateT"] = np.ascontiguousarray(f(b_exp_gate[0]).reshape(NE, 8, 128).transpose(2, 0, 1))
    shared["w_exp_up"] = f(w_exp_up[0])
    shared["b_upT"] = np.ascontiguousarray(f(b_exp_up[0]).reshape(NE, 8, 128).transpose(2, 0, 1))
    shared["w_exp_down"] = f(w_exp_down[0])
    shared["b_exp_down"] = f(b_exp_down[0])
    shared["norm_final_g"] = f(norm_final_g).reshape(1, -1)
    x = np.asarray(x, dtype=np.float32)
    c = np.asarray(c, dtype=np.float32)
    in_maps = []
    for ci in range(8):
        b, s = ci // 4, ci % 4
        xs = x[b, s * TOK:(s + 1) * TOK]
        m = dict(shared)
        m["x"] = np.ascontiguousarray(xs)
        m["xT"] = np.ascontiguousarray(xs.T)
        m["cT"] = fm(c[b])
        xp = np.zeros((D, 3 * TOK), np.float32)
        pm = np.zeros((128, 4), np.float32)
        for j in range(3):
            sj = s - 3 + j
            if sj >= 0:
                xp[:, j * TOK:(j + 1) * TOK] = x[b, sj * TOK:(sj + 1) * TOK].T
                pm[:, j] = 1.0
        m["xpT"] = xp
        m["pmask"] = pm
        in_maps.append(m)
    return in_maps


def kernel(**inputs):
    in_maps = prep_inputs(**inputs)
    nc = build_nc()
    names = set()
    for alloc in nc.allocations:
        if isinstance(alloc, mybir.MemoryLocationSet) and alloc.kind == "ExternalInput":
            names.add(alloc.memorylocations[0].name)
    in_maps = [{k: v for k, v in m.items() if k in names} for m in in_maps]
    res = run_bass_kernel_spmd(nc, in_maps, core_ids=list(range(8)))
    out = np.zeros((2, 4 * TOK, D), np.float32)
    for ci in range(8):
        b, s = ci // 4, ci % 4
        out[b, s * TOK:(s + 1) * TOK] = res.results[ci]["out"]
    return out
```

```python
import numpy as np
from contextlib import ExitStack
import concourse.bass as bass
import concourse.mybir as mybir
from concourse.bass_utils import run_bass_kernel_spmd

F32 = mybir.dt.float32
BF16 = mybir.dt.bfloat16
U32 = mybir.dt.uint32
AF = mybir.ActivationFunctionType
ALU = mybir.AluOpType

D = 4096
TOK = 2048
TT = 512
NTT = TOK // TT
NPRE = 3 * NTT
KD = D // 128
EPS = 1e-5
OFF_Q, OFF_K, OFF_V, OFF_G, OFF_A, OFF_U, OFF_Z, OFF_GA, OFF_GB = 0, 1024, 2048, 4096, 6144, 6160, 8208, 10256, 14352
NE = 32
CAP = 512
DE = 1024
DEBUG = False
STAGE = 2


class Prog:
    ENG = ["pe", "act", "dve", "pool", "sp"]

    def __init__(self, nc, es, nlanes=20):
        self.nc = nc
        self.eng = {"pe": nc.tensor, "act": nc.scalar, "dve": nc.vector, "pool": nc.gpsimd, "sp": nc.sync}
        self.semh = {e: es.enter_context(nc.semaphore("sem_" + e)) for e in self.ENG}
        self.cnt = {e: 0 for e in self.ENG}
        self.lanes = {}
        for q in ("sp", "pool"):
            self.lanes[q] = [0] * nlanes
            for i in range(nlanes):
                self.semh[("lane", q, i)] = es.enter_context(nc.semaphore(f"ln_{q}_{i}"))
        self.lane_rr = {"sp": 0, "pool": 0}
        self.waited = {e: {} for e in self.ENG}
        self.lastw = {}
        self.readers = {}
        self.npe = 0
        self.marks = []

    def _wait(self, e, tickets):
        for (k, v) in tickets:
            if v <= 0:
                continue
            if self.waited[e].get(k, 0) >= v:
                continue
            self.waited[e][k] = v
            self.eng[e].wait_ge(self.semh[k], v)

    def _deps(self, e, reads, writes):
        need = {}

        def add(t):
            if t is None:
                return
            k, v = t
            if e == "pe" and k == "pe":
                return
            if need.get(k, 0) < v:
                need[k] = v
        for b in reads:
            add(self.lastw.get(b))
        for b in writes:
            add(self.lastw.get(b))
            for k, v in self.readers.get(b, {}).items():
                add((k, v))
        return list(need.items())

    def _commit(self, ticket, reads, writes):
        k, v = ticket
        for b in reads:
            r = self.readers.setdefault(b, {})
            if r.get(k, 0) < v:
                r[k] = v
        for b in writes:
            self.lastw[b] = ticket
            self.readers[b] = {}

    def mark(self, name):
        self.marks.append((name, self.npe))

    def op(self, e, fn, reads=(), writes=(), inc=True):
        if e == "pe":
            self.npe += 1
        self._wait(e, self._deps(e, reads, writes))
        ins = fn(self.eng[e])
        if inc:
            self.cnt[e] += 1
            ins.then_inc(self.semh[e], 1)
            t = (e, self.cnt[e])
        else:
            t = (e, self.cnt[e] + 1)
        self._commit(t, reads, writes)

    def dma(self, q, fn, reads=(), writes=()):
        lanes = self.lanes[q]
        i = self.lane_rr[q]
        self.lane_rr[q] = (i + 1) % len(lanes)
        key = ("lane", q, i)
        deps = self._deps(q, reads, writes)
        deps.append((key, lanes[i]))
        self._wait(q, deps)
        ins = fn(self.eng[q])
        lanes[i] += 16
        ins.then_inc(self.semh[key], 16)
        self._commit((key, lanes[i]), reads, writes)

    def barrier(self, pool=True):
        engs = [e for e in self.ENG if pool or e != "pool"]
        ts = [(e, self.cnt[e]) for e in engs]
        for q in self.lanes:
            if q == "pool" and not pool:
                continue
            for i, v in enumerate(self.lanes[q]):
                ts.append((("lane", q, i), v))
        for e in engs:
            self._wait(e, [t for t in ts if t[0] != e])


_uid = [0]


def build_nc():
    nc = bass.Bass("TRN2", target_bir_lowering=False)

    def din(name, shape, dt=F32):
        return nc.dram_tensor(name, list(shape), dt, kind="ExternalInput").ap()

    def dscr(name, shape, dt):
        return nc.dram_tensor(name, list(shape), dt).ap()

    I = {}
    I["xT"] = din("xT", [D, TOK])
    I["xpT"] = din("xpT", [D, NPRE * TT])
    I["pmask"] = din("pmask", [128, 4])
    I["cT"] = din("cT", [128, KD])
    I["consts"] = din("consts", [128, 7 * 128])
    I["w_ada"] = din("w_ada", [D, 6 * D])
    I["b_adaT"] = din("b_adaT", [128, 6 * KD])
    I["gmixT"] = din("gmixT", [128, KD])
    I["w_in"] = din("w_in", [D, 18448])
    I["w_up_ext"] = din("w_up_ext", [32, 1024])
    I["gla_norm_g"] = din("gla_norm_g", [1, 512])
    I["sgu_ln_g"] = din("sgu_ln_g", [1, 2048])
    I["sgu_ln_b"] = din("sgu_ln_b", [1, 2048])
    I["wsT"] = din("wsT", [128, 8, 128])
    I["b_spatial"] = din("b_spatial", [1, 8 * 128])
    I["w_branch_a"] = din("w_branch_a", [2048, D])
    I["w_branch_b"] = din("w_branch_b", [2048, D])
    I["w_out"] = din("w_out", [D, D])
    I["gffnT"] = din("gffnT", [128, KD])
    I["w_routerT"] = din("w_routerT", [128, KD, NE])
    if STAGE >= 2:
        I["w_exp_gate"] = din("w_exp_gate", [NE, D, DE])
        I["b_gateT"] = din("b_gateT", [128, NE, 8])
        I["w_exp_up"] = din("w_exp_up", [NE, D, DE])
        I["b_upT"] = din("b_upT", [128, NE, 8])
        I["w_exp_down"] = din("w_exp_down", [NE, DE, D])
        I["b_exp_down"] = din("b_exp_down", [NE, D])
        I["norm_final_g"] = din("norm_final_g", [1, D])
        I["norm_ffn_g"] = din("norm_ffn_g", [1, D])
        I["b_router"] = din("b_router", [1, NE])
        I["b_ada_row"] = din("b_ada_row", [1, 6 * D])
    out = nc.dram_tensor("out", [TOK, D], F32, kind="ExternalOutput").ap()
    dbg = None
    if DEBUG:
        dbg = nc.dram_tensor("dbg", [TOK, D], F32, kind="ExternalOutput").ap()

    mod_d = dscr("mod_d", [1, 6 * D], F32)
    o_d = dscr("o_d", [TOK, 2048], F32)
    x1_d = dscr("x1_d", [TOK, D], F32)
    if STAGE >= 2:
        Xd = dscr("Xd", [NE * CAP, D], BF16)
        Yd = dscr("Yd", [NE * CAP, D], F32)

    with ExitStack() as es:
        P = Prog(nc, es)

        def sb(es_, name, shape, dt):
            _uid[0] += 1
            return es_.enter_context(nc.sbuf_tensor(f"{name}_{_uid[0]}", list(shape), dt))

        def ps(es_, name, shape, dt):
            _uid[0] += 1
            return es_.enter_context(nc.psum_tensor(f"{name}_{_uid[0]}", list(shape), dt))

        cst = sb(es, "cst", [128, 7 * 128], F32)
        P.dma("sp", lambda q: q.dma_start(out=cst[:], in_=I["consts"][:, :]), writes=["cst"])
        ident_f = cst[:, 0:128]
        ones_f = cst[:, 128:256]
        triC = cst[:, 256:384]
        triR = cst[:, 384:512]
        maskT = cst[:, 512:640]
        ident_b_t = sb(es, "identb", [128, 128], BF16)
        ones_b_t = sb(es, "onesb", [128, 128], BF16)
        P.op("dve", lambda v: v.tensor_copy(out=ident_b_t[:], in_=ident_f), reads=["cst"], writes=["identb"])
        P.op("dve", lambda v: v.tensor_copy(out=ones_b_t[:], in_=ones_f), reads=["cst"], writes=["onesb"])
        ident_b = ident_b_t[:]
        modT = sb(es, "modT", [128, 6 * KD], F32)
        gm1T = sb(es, "gm1T", [128, KD], F32)
        gm2T = sb(es, "gm2T", [128, KD], F32)
        eS = ExitStack()
        S = sb(eS, "S", [128, 8, 512], F32)
        Sbf = sb(eS, "Sbf", [128, 8, 512], BF16)

        with ExitStack() as e0:
            cT = sb(e0, "cT", [128, KD], F32)
            scT = sb(e0, "scT", [128, KD], BF16)
            P.dma("sp", lambda q: q.dma_start(out=cT[:], in_=I["cT"][:, :]), writes=["cT"])
            P.op("act", lambda a: a.activation(out=scT[:], in_=cT[:], func=AF.Silu), reads=["cT"], writes=["scT"])
            NWB = 3
            wbuf = [sb(e0, f"wada{i}", [128, 8, 512], BF16) for i in range(NWB)]
            mps = [ps(e0, f"modps{i}", [128, 512], F32) for i in range(2)]
            row = [sb(e0, f"modrow{i}", [1, 512], F32) for i in range(2)]
            wv = I["w_ada"].rearrange("(k p) c -> p k c", p=128)
            it = 0
            for cb in range(48):
                pb = cb % 2
                for kk in range(4):
                    b = it % NWB
                    it += 1
                    P.dma("pool", lambda q, b=b, kk=kk, cb=cb: q.dma_start(
                        out=wbuf[b][:], in_=wv[:, kk * 8:(kk + 1) * 8, cb * 512:(cb + 1) * 512]),
                        writes=[("wada", b)])
                    for k8 in range(8):
                        k = kk * 8 + k8
                        P.op("pe", lambda pe, b=b, k=k, k8=k8, pb=pb: pe.matmul(
                            mps[pb][0:1, :], lhsT=scT[:, k:k + 1], rhs=wbuf[b][:, k8, :],
                            start=(k == 0), stop=(k == KD - 1)),
                            reads=["scT", ("wada", b)], writes=[("modps", pb)], inc=(k8 == 7))
                P.op("dve", lambda v, pb=pb: v.tensor_copy(out=row[pb][:], in_=mps[pb][0:1, :]),
                     reads=[("modps", pb)], writes=[("modrow", pb)])
                P.dma("sp", lambda q, pb=pb, cb=cb: q.dma_start(
                    out=mod_d[0:1, cb * 512:(cb + 1) * 512], in_=row[pb][:]),
                    reads=[("modrow", pb)], writes=[("mod_d", cb)])
            P.barrier()
        with ExitStack() as e0:
            t1 = sb(e0, "t1", [128, 6 * KD], F32)
            t2 = sb(e0, "t2", [128, KD], F32)
            t3 = sb(e0, "t3", [128, KD], F32)
            P.dma("sp", lambda q: q.dma_start(out=t1[:], in_=mod_d.rearrange("o (j p) -> p (o j)", p=128),
                                              allow_slow_non_contiguous=True), writes=["t1"])
            P.dma("sp", lambda q: q.dma_start(out=modT[:], in_=I["b_adaT"][:, :]), writes=["modT"])
            P.dma("sp", lambda q: q.dma_start(out=t2[:], in_=I["gmixT"][:, :]), writes=["t2"])
            P.dma("sp", lambda q: q.dma_start(out=t3[:], in_=I["gffnT"][:, :]), writes=["t3"])
            P.op("dve", lambda v: v.tensor_tensor(out=modT[:], in0=modT[:], in1=t1[:], op=ALU.add),
                 reads=["modT", "t1"], writes=["modT"])
            P.op("dve", lambda v: v.scalar_tensor_tensor(out=gm1T[:], in0=modT[:, KD:2 * KD], scalar=1.0, in1=t2[:],
                                                         op0=ALU.add, op1=ALU.mult),
                 reads=["modT", "t2"], writes=["gm1T"])
            P.op("dve", lambda v: v.scalar_tensor_tensor(out=gm2T[:], in0=modT[:, 4 * KD:5 * KD], scalar=1.0, in1=t3[:],
                                                         op0=ALU.add, op1=ALU.mult),
                 reads=["modT", "t3"], writes=["gm2T"])
            P.barrier()
        sh1T = modT[:, 0:KD]
        g1T = modT[:, 2 * KD:3 * KD]
        sh2T = modT[:, 3 * KD:4 * KD]

        w_in_v = I["w_in"].rearrange("(k p) c -> p k c", p=128)
        xT_v = I["xT"].rearrange("(k p) t -> p k t", p=128)
        xpT_v = I["xpT"].rearrange("(k p) t -> p k t", p=128)

        def hT_steps(xview, col0, hT, hkeys, xb, sq, rbc, ssp, sskey):
            for k in range(KD):
                b = k % len(xb)
                P.dma("sp", lambda q, b=b, k=k: q.dma_start(out=xb[b][:], in_=xview[:, k, col0:col0 + TT]),
                      writes=[("xb", b)])
                P.op("act", lambda a, b=b, k=k: a.activation(out=sq[k % 2][:], in_=xb[b][:], func=AF.Square),
                     reads=[("xb", b)], writes=[("sq", k % 2)])
                P.op("pe", lambda pe, k=k: pe.matmul(ssp[:], lhsT=ones_f, rhs=sq[k % 2][:],
                                                     start=(k == 0), stop=(k == KD - 1)),
                     reads=["cst", ("sq", k % 2)], writes=[sskey])
                yield
            P.op("dve", lambda v: v.tensor_scalar(out=rbc[:], in0=ssp[:], scalar1=1.0 / D, scalar2=EPS,
                                                  op0=ALU.mult, op1=ALU.add), reads=[sskey], writes=["rbc"])
            P.op("act", lambda a: a.activation(out=rbc[:], in_=rbc[:], func=AF.Sqrt), reads=["rbc"], writes=["rbc"])
            P.op("dve", lambda v: v.reciprocal(out=rbc[:], in_=rbc[:]), reads=["rbc"], writes=["rbc"])
            yield
            for k in range(KD):
                b = k % len(xb)
                P.dma("sp", lambda q, b=b, k=k: q.dma_start(out=xb[b][:], in_=xview[:, k, col0:col0 + TT]),
                      writes=[("xb", b)])
                P.op("dve", lambda v, b=b, k=k: v.tensor_tensor(out=sq[k % 2][:], in0=xb[b][:], in1=rbc[:], op=ALU.mult),
                     reads=[("xb", b), "rbc"], writes=[("sq", k % 2)])
                P.op("act", lambda a, k=k: a.activation(out=hT[:, k, :], in_=sq[k % 2][:], func=AF.Identity,
                                                        scale=gm1T[:, k:k + 1], bias=sh1T[:, k:k + 1]),
                     reads=[("sq", k % 2), "gm1T", "modT"], writes=[hkeys[k]])
                yield

        pend = [None]

        def defer(post):
            old = pend[0]
            pend[0] = post
            if old is not None:
                old()

        def flush():
            if pend[0] is not None:
                pend[0]()
                pend[0] = None

        def step(gen, n):
            if gen is None:
                return
            for _ in range(n):
                if next(gen, "done") == "done":
                    return

        def compute_hT(esx, xview, col0, hT, ssp, sskey):
            with ExitStack() as e1:
                xb = [sb(e1, f"xb{i}", [128, TT], F32) for i in range(3)]
                sq = [sb(e1, f"sq{i}", [128, TT], F32) for i in range(2)]
                rbc = sb(e1, "rbc", [128, TT], F32)
                step(hT_steps(xview, col0, hT, hT_keys, xb, sq, rbc, ssp, sskey), 1000)
                P.barrier(pool=False)

        hT_keys = [("hT", k) for k in range(KD)]

        class WCache:
            def __init__(self, name, nblk, nk):
                self.name = name
                self.ap = dscr(name, [nblk, 128, nk, 128], BF16)
                self.idx = {}
                self.done = set()

            def slot(self, c0):
                if c0 not in self.idx:
                    self.idx[c0] = len(self.idx)
                return self.idx[c0]

        class WStream:
            def __init__(self, esx, name, nbuf, kmax):
                self.name = name
                self.bufs = [sb(esx, f"{name}{i}", [128, kmax, 128], BF16) for i in range(nbuf)]
                self.i = 0

            def load(self, wview, c0, ncols, nk, cache=None):
                b = self.i % len(self.bufs)
                self.i += 1
                t = self.bufs[b]
                key = (self.name, b)
                if cache is not None:
                    ci = cache.slot(c0)
                    ck = (cache.name, ci)
                    if ci in cache.done:
                        P.dma("pool", lambda q: q.dma_start(out=t[:, 0:nk, 0:ncols], in_=cache.ap[ci][:, 0:nk, 0:ncols]),
                              reads=[ck], writes=[key])
                        return t, key
                P.dma("pool", lambda q: q.dma_start(out=t[:, 0:nk, 0:ncols], in_=wview[:, 0:nk, c0:c0 + ncols]),
                      writes=[key])
                if cache is not None:
                    P.dma("sp", lambda q: q.dma_start(out=cache.ap[ci][:, 0:nk, 0:ncols], in_=t[:, 0:nk, 0:ncols]),
                          reads=[key], writes=[ck])
                    cache.done.add(ci)
                return t, key

        wc_in = WCache("wc_in", 145, KD)
        wc_a = WCache("wc_a", 32, 16)
        wc_b = WCache("wc_b", 32, 16)
        wc_o = WCache("wc_o", 32, KD)

        def proj(wt, wkey, ncols, nk, rhsT, rhs_keys, pst, pskey):
            for k in range(nk):
                P.op("pe", lambda pe, k=k: pe.matmul(pst[0:ncols, :], lhsT=wt[:, k, 0:ncols], rhs=rhsT[:, k, :],
                                                     start=(k == 0), stop=(k == nk - 1)),
                     reads=[wkey, rhs_keys[k]], writes=[pskey], inc=(k == nk - 1))

        P.mark('1A')
        P.op("dve", lambda v: v.memset(S[:], 0.0), writes=["S"])
        P.op("dve", lambda v: v.memset(Sbf[:], 0.0), writes=["Sbf"])
        with ExitStack() as eA:
            wup = sb(eA, "wup", [32, 1024], F32)
            pmk = sb(eA, "pmk", [128, 4], F32)
            P.dma("sp", lambda q: q.dma_start(out=pmk[:], in_=I["pmask"][:, :]), writes=["pmk"])
            P.dma("sp", lambda q: q.dma_start(out=wup[:], in_=I["w_up_ext"][:, :]), writes=["wup"])
            hTs = [sb(eA, f"hT{i}", [128, KD, TT], BF16) for i in range(2)]
            hTk = [[("hT", i, k) for k in range(KD)] for i in range(2)]
            xb_ = [sb(eA, f"xbA{i}", [128, TT], F32) for i in range(2)]
            sq_ = [sb(eA, f"sqA{i}", [128, TT], F32) for i in range(2)]
            rbc_ = sb(eA, "rbcA", [128, TT], F32)
            WS = WStream(eA, "wsA", 3, KD)
            a_ext = sb(eA, "a_ext", [32, TT], F32)
            spt = sb(eA, "spt", [128, 4, 1024], F32)
            Ecum = sb(eA, "Ecum", [128, TT], F32)
            Ek = sb(eA, "Ek", [128, TT], F32)
            Eq = sb(eA, "Eq", [128, TT], F32)
            qabs = sb(eA, "qabs", [128, 8, TT], BF16)
            qrel = sb(eA, "qrel", [128, 8, TT], BF16)
            krel = sb(eA, "krel", [128, 8, TT], BF16)
            kreltok = sb(eA, "kreltok", [128, 4, 1024], BF16)
            vtok = sb(eA, "vtok", [128, 4, 2048], BF16)
            vtmp = [sb(eA, f"vtmp{i}", [128, TT], BF16) for i in range(2)]
            dec = sb(eA, "dec", [128, 8, 4], F32)
            sT = [sb(eA, f"sT{i}", [128, 128], BF16) for i in range(2)]
            ost = [sb(eA, f"ost{i}", [128, 512], F32) for i in range(1)]
            pacc = [ps(eA, f"pacc{i}", [128, 512], F32) for i in range(2)]
            pC = ps(eA, "pC", [128, 512], F32)
            pR = ps(eA, "pR", [128, 512], F32)
            pT = [ps(eA, "pT0", [128, 1024], BF16)]
            pSS = ps(eA, "pSS", [128, 512], F32)
            pG = [ps(eA, f"pG{i}", [128, 512], F32) for i in range(2)]
            nacc = [0]

            def next_acc():
                i = nacc[0] % 2
                nacc[0] += 1
                return pacc[i], ("pacc", i)
            ntr = [0]

            def next_tr():
                return pT[0], ("pT", 0)

            def tile_src(it_):
                return (xpT_v, it_ * TT) if it_ < NPRE else (xT_v, (it_ - NPRE) * TT)

            def hT_gen(it_):
                if it_ >= NPRE + NTT:
                    return None
                xv, c0_ = tile_src(it_)
                return hT_steps(xv, c0_, hTs[it_ % 2], hTk[it_ % 2], xb_, sq_, rbc_, pSS, "pSS")

            step(hT_gen(0), 1000)
            for it in range(NPRE + NTT):
                P.mark(f'1A_t{it}')
                prefix = it < NPRE
                tt = it - NPRE
                hT = hTs[it % 2]
                hT_keys = hTk[it % 2]
                bg = hT_gen(it + 1)
                wt, wk = WS.load(w_in_v, OFF_A, 16, KD, wc_in)
                pa, pk = next_acc()
                proj(wt, wk, 16, KD, hT, hT_keys, pa, pk)
                step(bg, 3)
                P.op("dve", lambda v: v.memset(a_ext[:], 1.0), writes=["a_ext"])
                P.op("dve", lambda v, pa=pa: v.tensor_copy(out=a_ext[0:16, :], in_=pa[0:16, :]),
                     reads=[pk, "a_ext"], writes=["a_ext"])
                for sub in range(4):
                    for hf in range(2):
                        P.op("pe", lambda pe, sub=sub, hf=hf: pe.matmul(
                            (pC if hf == 0 else pR)[:], lhsT=a_ext[:, sub * 128:(sub + 1) * 128],
                            rhs=wup[:, hf * 512:(hf + 1) * 512], start=True, stop=True),
                            reads=["a_ext", "wup"], writes=["pC" if hf == 0 else "pR"])
                        P.op("act", lambda a, sub=sub, hf=hf: a.activation(
                            out=spt[:, sub, hf * 512:(hf + 1) * 512], in_=(pC if hf == 0 else pR)[:],
                            func=AF.Exp, scale=-1.0),
                            reads=["pC" if hf == 0 else "pR"], writes=[("spt", sub, hf)])
                        P.op("act", lambda a, sub=sub, hf=hf: a.activation(
                            out=spt[:, sub, hf * 512:(hf + 1) * 512], in_=spt[:, sub, hf * 512:(hf + 1) * 512],
                            func=AF.Ln, bias=1.0, scale=1.0),
                            reads=[("spt", sub, hf)], writes=[("spt", sub, hf)])
                for kc in range(8):
                    hf = kc // 4
                    for sub in range(4):
                        P.op("pe", lambda pe, sub=sub, kc=kc: pe.matmul(
                            pC[:, sub * 128:(sub + 1) * 128], lhsT=spt[:, sub, kc * 128:(kc + 1) * 128], rhs=triC,
                            start=True, stop=True), reads=[("spt", sub, hf), "cst"], writes=["pC"])
                        P.op("pe", lambda pe, sub=sub, kc=kc: pe.matmul(
                            pR[:, sub * 128:(sub + 1) * 128], lhsT=spt[:, sub, kc * 128:(kc + 1) * 128], rhs=triR,
                            start=True, stop=True), reads=[("spt", sub, hf), "cst"], writes=["pR"])
                    P.op("act", lambda a: a.activation(out=Ecum[:], in_=pC[:], func=AF.Exp), reads=["pC"], writes=["Ecum"])
                    P.op("act", lambda a: a.activation(out=Ek[:], in_=pR[:], func=AF.Exp), reads=["pR"], writes=["Ek"])
                    if not prefix:
                        P.op("act", lambda a: a.activation(out=Eq[:], in_=pR[:], func=AF.Exp, scale=-1.0),
                             reads=["pR"], writes=["Eq"])
                    for c in range(4):
                        P.op("dve", lambda v, c=c, kc=kc: v.tensor_copy(
                            out=dec[:, kc, c:c + 1], in_=Ecum[:, c * 128 + 127:c * 128 + 128]),
                            reads=["Ecum"], writes=[("dec", kc)])
                    if not prefix:
                        wt, wk = WS.load(w_in_v, OFF_Q + kc * 128, 128, KD, wc_in)
                        pa, pk = next_acc()
                        proj(wt, wk, 128, KD, hT, hT_keys, pa, pk)
                        step(bg, 3)
                        P.op("dve", lambda v, pa=pa, kc=kc: v.scalar_tensor_tensor(
                            out=qabs[:, kc, :], in0=pa[:], scalar=1.0 / 16.0, in1=Ecum[:], op0=ALU.mult, op1=ALU.mult),
                            reads=[pk, "Ecum"], writes=[("qabs", kc)])
                        P.op("dve", lambda v, pa=pa, kc=kc: v.scalar_tensor_tensor(
                            out=qrel[:, kc, :], in0=pa[:], scalar=1.0 / 16.0, in1=Eq[:], op0=ALU.mult, op1=ALU.mult),
                            reads=[pk, "Eq"], writes=[("qrel", kc)])
                    wt, wk = WS.load(w_in_v, OFF_K + kc * 128, 128, KD, wc_in)
                    pa, pk = next_acc()
                    proj(wt, wk, 128, KD, hT, hT_keys, pa, pk)
                    step(bg, 3)
                    P.op("dve", lambda v, pa=pa, kc=kc: v.tensor_tensor(out=krel[:, kc, :], in0=pa[:], in1=Ek[:], op=ALU.mult),
                         reads=[pk, "Ek"], writes=[("krel", kc)])
                    def post_k(kc=kc):
                        tp, tk = next_tr()
                        for sub in range(4):
                            P.op("pe", lambda pe, sub=sub, kc=kc, tp=tp: pe.transpose(
                                out=tp[:, sub * 128:(sub + 1) * 128], in_=krel[:, kc, sub * 128:(sub + 1) * 128], identity=ident_b),
                                reads=[("krel", kc), "identb"], writes=[tk], inc=(sub == 3))
                        P.op("act", lambda a, kc=kc, tp=tp: a.activation(
                            out=kreltok[:, :, kc * 128:(kc + 1) * 128],
                            in_=tp[:, 0:512].rearrange("p (s c) -> p s c", s=4), func=AF.Copy),
                            reads=[tk], writes=[("kreltok", kc)])
                    defer(post_k)
                for vc in range(16):
                    wt, wk = WS.load(w_in_v, OFF_V + vc * 128, 128, KD, wc_in)
                    pa, pk = next_acc()
                    proj(wt, wk, 128, KD, hT, hT_keys, pa, pk)
                    step(bg, 3)
                    vb = vc % 2
                    P.op("act", lambda a, pa=pa, vb=vb: a.activation(out=vtmp[vb][:], in_=pa[:], func=AF.Copy),
                         reads=[pk], writes=[("vtmp", vb)])
                    def post_v(vc=vc, vb=vb):
                        tp, tk = next_tr()
                        for sub in range(4):
                            P.op("pe", lambda pe, sub=sub, vb=vb, tp=tp: pe.transpose(
                                out=tp[:, sub * 128:(sub + 1) * 128], in_=vtmp[vb][:, sub * 128:(sub + 1) * 128], identity=ident_b),
                                reads=[("vtmp", vb), "identb"], writes=[tk], inc=(sub == 3))
                        P.op("dve", lambda v, vc=vc, tp=tp: v.tensor_copy(
                            out=vtok[:, :, vc * 128:(vc + 1) * 128], in_=tp[:, 0:512].rearrange("p (s c) -> p s c", s=4)),
                            reads=[tk], writes=[("vtok", vc // 4)])
                    defer(post_v)
                flush()
                for c in range(4):
                    for h in range(4):
                        k0, k1 = 2 * h, 2 * h + 1
                        if not prefix:
                            sp_ = pC if h % 2 == 0 else pR
                            skey = "pC" if h % 2 == 0 else "pR"
                            cs = slice(c * 128, (c + 1) * 128)
                            for i, kc in enumerate((k0, k1)):
                                P.op("pe", lambda pe, kc=kc, i=i, sp_=sp_, cs=cs: pe.matmul(
                                    sp_[:, 0:128], lhsT=krel[:, kc, cs], rhs=qrel[:, kc, cs], start=(i == 0), stop=(i == 1)),
                                    reads=[("krel", kc), ("qrel", kc)], writes=[skey], inc=(i == 1))
                            sb_ = sT[h % 2]
                            P.op("dve", lambda v, sb_=sb_, sp_=sp_: v.tensor_tensor(out=sb_[:], in0=sp_[:, 0:128], in1=maskT, op=ALU.mult),
                                 reads=[skey, "cst"], writes=[("sT", h % 2)])
                            po, pok = next_acc()
                            P.op("pe", lambda pe, po=po, sb_=sb_, c=c, h=h: pe.matmul(
                                po[:], lhsT=sb_[:], rhs=vtok[:, c, h * 512:(h + 1) * 512], start=True, stop=False),
                                reads=[("sT", h % 2), ("vtok", h)], writes=[pok], inc=False)
                            for i, kc in enumerate((k0, k1)):
                                P.op("pe", lambda pe, po=po, kc=kc, i=i, cs=cs: pe.matmul(
                                    po[:], lhsT=qabs[:, kc, cs], rhs=Sbf[:, kc, :], start=False, stop=(i == 1)),
                                    reads=[("qabs", kc), ("Sbf", kc)], writes=[pok], inc=(i == 1))
                            ob = ost[0]
                            P.op("act", lambda a, ob=ob, po=po: a.activation(out=ob[:], in_=po[:], func=AF.Copy),
                                 reads=[pok], writes=[("ost", 0)])
                            r0 = tt * TT + c * 128
                            P.dma("sp", lambda q, ob=ob, r0=r0, h=h: q.dma_start(
                                out=o_d[r0:r0 + 128, h * 512:(h + 1) * 512], in_=ob[:]),
                                reads=[("ost", 0)], writes=[("o_d", tt, c, h)])
                        for i, kc in enumerate((k0, k1)):
                            pg = pG[i]
                            P.op("pe", lambda pe, pg=pg, kc=kc, c=c, h=h: pe.matmul(
                                pg[:], lhsT=kreltok[:, c, kc * 128:(kc + 1) * 128], rhs=vtok[:, c, h * 512:(h + 1) * 512],
                                start=True, stop=True),
                                reads=[("kreltok", kc), ("vtok", h)], writes=[("pG", i)])
                            P.op("dve", lambda v, pg=pg, kc=kc, c=c: v.scalar_tensor_tensor(
                                out=S[:, kc, :], in0=S[:, kc, :], scalar=dec[:, kc, c:c + 1], in1=pg[:],
                                op0=ALU.mult, op1=ALU.add),
                                reads=[("S", kc), ("dec", kc), ("pG", i)], writes=[("S", kc)])
                            P.op("act", lambda a, kc=kc: a.activation(out=Sbf[:, kc, :], in_=S[:, kc, :], func=AF.Copy),
                                 reads=[("S", kc)], writes=[("Sbf", kc)])
                if prefix and it % NTT == NTT - 1:
                    j_ = it // NTT
                    for kc in range(8):
                        P.op("dve", lambda v, kc=kc, j_=j_: v.tensor_scalar(out=S[:, kc, :], in0=S[:, kc, :], scalar1=pmk[:, j_:j_ + 1],
                                                                          scalar2=None, op0=ALU.mult),
                             reads=[("S", kc), "pmk"], writes=[("S", kc)])
                        P.op("act", lambda a, kc=kc: a.activation(out=Sbf[:, kc, :], in_=S[:, kc, :], func=AF.Copy),
                             reads=[("S", kc)], writes=[("Sbf", kc)])
                step(bg, 1000)
            P.barrier()

        P.barrier()
        eS.close()

        P.mark('1B')
        lgT = sb(es, "lgT", [32, TOK], F32)
        wa_v = I["w_branch_a"].rearrange("(k p) c -> p k c", p=128)
        wb_v = I["w_branch_b"].rearrange("(k p) c -> p k c", p=128)
        wo_v = I["w_out"].rearrange("(k p) c -> p k c", p=128)
        with ExitStack() as eB:
            hT = sb(eB, "hTb", [128, KD, TT], BF16)
            ogT = sb(eB, "ogT", [128, 16, TT], BF16)
            suT = sb(eB, "suT", [128, 16, TT], BF16)
            WS = WStream(eB, "wsB", 4, KD)
            gn_bc = sb(eB, "gn_bc", [128, 512], F32)
            P.dma("sp", lambda q: q.dma_start(out=gn_bc[:], in_=I["gla_norm_g"][0, :].partition_broadcast(128)),
                  writes=["gn_bc"])
            wsT_b = sb(eB, "wsT_b", [128, 8, 128], BF16)
            eW = ExitStack()
            wsT_f = sb(eW, "wsT_f", [128, 8, 128], F32)
            P.dma("sp", lambda q: q.dma_start(out=wsT_f[:], in_=I["wsT"][:, :, :]), writes=["wsT_f"])
            for g in range(8):
                P.op("dve", lambda v, g=g: v.tensor_tensor(out=wsT_b[:, g, :], in0=wsT_f[:, g, :], in1=maskT, op=ALU.mult),
                     reads=["wsT_f", "cst"], writes=["wsT_b"])
            P.barrier()
            eW.close()
            bsp = sb(eB, "bsp", [1, 1024], F32)
            bhi = sb(eB, "bhi", [1, 1024], BF16)
            blo = sb(eB, "blo", [1, 1024], BF16)
            P.dma("sp", lambda q: q.dma_start(out=bsp[:], in_=I["b_spatial"][:, :]), writes=["bsp"])
            P.op("dve", lambda v: v.tensor_copy(out=bhi[:], in_=bsp[:]), reads=["bsp"], writes=["bhi"])
            P.op("dve", lambda v: v.tensor_tensor(out=bsp[:], in0=bsp[:], in1=bhi[:], op=ALU.subtract),
                 reads=["bsp", "bhi"], writes=["bsp"])
            P.op("dve", lambda v: v.tensor_copy(out=blo[:], in_=bsp[:]), reads=["bsp"], writes=["blo"])
            wr = sb(eB, "wr", [128, KD, NE], F32)
            P.dma("sp", lambda q: q.dma_start(out=wr[:], in_=I["w_routerT"][:, :, :]), writes=["wr"])
            for k in range(KD):
                P.op("dve", lambda v, k=k: v.tensor_scalar(out=wr[:, k, :], in0=wr[:, k, :], scalar1=gm2T[:, k:k + 1],
                                                           scalar2=None, op0=ALU.mult), reads=["wr", "gm2T"], writes=["wr"])
            pacc = [ps(eB, f"pb_acc{i}", [128, 512], F32) for i in range(4)]
            pT = [ps(eB, f"pb_T{i}", [128, 1024], BF16) for i in range(2)]
            pX = ps(eB, "pb_X", [128, 512], F32)
            pL = ps(eB, "pb_L", [128, 512], F32)
            nacc = [0]

            def next_acc():
                i = nacc[0] % 4
                nacc[0] += 1
                return pacc[i], ("pb_acc", i)
            ntr = [0]

            def next_tr():
                i = ntr[0] % 2
                ntr[0] += 1
                return pT[i], ("pb_T", i)

            def gelu_from_psum(esx, pa, pk, outap, outkey, tmps, sfx=0):
                xs, t = tmps
                kx, kt = ("g_xs", sfx), ("g_t", sfx)
                P.op("act", lambda a: a.activation(out=xs[:], in_=pa[:], func=AF.Copy), reads=[pk], writes=[kx])
                P.op("dve", lambda v: v.tensor_tensor(out=t[:], in0=xs[:], in1=xs[:], op=ALU.mult), reads=[kx], writes=[kt])
                P.op("dve", lambda v: v.tensor_scalar(out=t[:], in0=t[:], scalar1=0.044715, scalar2=1.0, op0=ALU.mult, op1=ALU.add),
                     reads=[kt], writes=[kt])
                P.op("dve", lambda v: v.tensor_tensor(out=t[:], in0=t[:], in1=xs[:], op=ALU.mult), reads=[kt, kx], writes=[kt])
                P.op("act", lambda a: a.activation(out=t[:], in_=t[:], func=AF.Sigmoid, scale=1.5957691216057308),
                     reads=[kt], writes=[kt])
                P.op("dve", lambda v: v.tensor_tensor(out=outap, in0=xs[:], in1=t[:], op=ALU.mult), reads=[kx, kt], writes=[outkey])

            for tt in range(NTT):
                P.mark(f'1B_t{tt}')
                compute_hT(eB, xT_v, tt * TT, hT, pX, "pb_X")
                with ExitStack() as e2:
                    on = sb(e2, "on", [128, 4, 2048], BF16)
                    ob = [sb(e2, f"ob{i}", [128, 2048], F32) for i in range(2)]
                    ss = sb(e2, "ss", [128, 4], F32)
                    junk = sb(e2, "junk", [128, 512], F32)
                    sg = [sb(e2, f"sg{i}", [128, TT], BF16) for i in range(2)]
                    for sub in range(4):
                        o_ = ob[sub % 2]
                        okey = ("ob", sub % 2)
                        r0 = tt * TT + sub * 128
                        P.dma("sp", lambda q, o_=o_, r0=r0: q.dma_start(out=o_[:], in_=o_d[r0:r0 + 128, :]),
                              reads=[("o_d", tt, sub, h_) for h_ in range(4)], writes=[okey])
                        for h in range(4):
                            hs = slice(h * 512, (h + 1) * 512)
                            P.op("act", lambda a, o_=o_, hs=hs, h=h: a.activation(out=junk[:], in_=o_[:, hs], func=AF.Square,
                                                                                  accum_out=ss[:, h:h + 1]),
                                 reads=[okey], writes=["junk", "ss"])
                        P.op("dve", lambda v: v.tensor_scalar(out=ss[:], in0=ss[:], scalar1=1.0 / 512, scalar2=EPS,
                                                              op0=ALU.mult, op1=ALU.add), reads=["ss"], writes=["ss"])
                        P.op("act", lambda a: a.activation(out=ss[:], in_=ss[:], func=AF.Sqrt), reads=["ss"], writes=["ss"])
                        P.op("dve", lambda v: v.reciprocal(out=ss[:], in_=ss[:]), reads=["ss"], writes=["ss"])
                        for h in range(4):
                            hs = slice(h * 512, (h + 1) * 512)
                            P.op("dve", lambda v, o_=o_, hs=hs, h=h, sub=sub: v.scalar_tensor_tensor(
                                out=on[:, sub, hs], in0=o_[:, hs], scalar=ss[:, h:h + 1], in1=gn_bc[:],
                                op0=ALU.mult, op1=ALU.mult), reads=[okey, "ss", "gn_bc"], writes=[("on", sub)])
                    for vc in range(16):
                        wt, wk = WS.load(w_in_v, OFF_G + vc * 128, 128, KD, wc_in)
                        pa, pk = next_acc()
                        proj(wt, wk, 128, KD, hT, hT_keys, pa, pk)
                        sgb = sg[vc % 2]
                        P.op("act", lambda a, sgb=sgb, pa=pa: a.activation(out=sgb[:], in_=pa[:], func=AF.Silu),
                             reads=[pk], writes=[("sg", vc % 2)])
                        tp, tk = next_tr()
                        for sub in range(4):
                            P.op("pe", lambda pe, sub=sub, vc=vc, tp=tp: pe.transpose(
                                out=tp[:, sub * 128:(sub + 1) * 128], in_=on[:, sub, vc * 128:(vc + 1) * 128], identity=ident_b),
                                reads=[("on", sub), "identb"], writes=[tk], inc=(sub == 3))
                        P.op("dve", lambda v, vc=vc, tp=tp, sgb=sgb: v.tensor_tensor(out=ogT[:, vc, :], in0=tp[:, 0:512], in1=sgb[:], op=ALU.mult),
                             reads=[tk, ("sg", vc % 2)], writes=[("ogT", vc)])
                    P.barrier(pool=False)
                P.mark(f'B3_{tt}')
                with ExitStack() as e3:
                    ztok = sb(e3, "ztok", [128, 4, 2048], BF16)
                    lng = sb(e3, "lng", [128, 2048], F32)
                    lnb = sb(e3, "lnb", [128, 2048], F32)
                    P.dma("sp", lambda q: q.dma_start(out=lng[:], in_=I["sgu_ln_g"][0, :].partition_broadcast(128)), writes=["lng"])
                    P.dma("sp", lambda q: q.dma_start(out=lnb[:], in_=I["sgu_ln_b"][0, :].partition_broadcast(128)), writes=["lnb"])
                    zn = sb(e3, "zn", [128, 4, 2048], BF16)
                    gx = [sb(e3, f"gx{i}", [128, TT], F32) for i in range(2)]
                    gt = [sb(e3, f"gt{i}", [128, TT], F32) for i in range(2)]
                    gz = [sb(e3, f"gz{i}", [128, TT], BF16) for i in range(2)]
                    zf = sb(e3, "zf", [128, 2048], F32)
                    st6 = sb(e3, "st6", [128, 4, 6], F32)
                    mv = sb(e3, "mv", [128, 2], F32)
                    rs = sb(e3, "rs", [128, 2], F32)
                    for zc in range(16):
                        wt, wk = WS.load(w_in_v, OFF_Z + zc * 128, 128, KD, wc_in)
                        pa, pk = next_acc()
                        proj(wt, wk, 128, KD, hT, hT_keys, pa, pk)
                        gzb = gz[zc % 2]
                        gelu_from_psum(e3, pa, pk, gzb[:], ("gz", zc % 2), (gx[zc % 2], gt[zc % 2]), zc % 2)
                        def post_z(zc=zc, gzb=gzb):
                            tp, tk = next_tr()
                            for sub in range(4):
                                P.op("pe", lambda pe, sub=sub, gzb=gzb, tp=tp: pe.transpose(
                                    out=tp[:, sub * 128:(sub + 1) * 128], in_=gzb[:, sub * 128:(sub + 1) * 128], identity=ident_b),
                                    reads=[("gz", zc % 2), "identb"], writes=[tk], inc=(sub == 3))
                            P.op("act", lambda a, zc=zc, tp=tp: a.activation(
                                out=ztok[:, :, zc * 128:(zc + 1) * 128], in_=tp[:, 0:512].rearrange("p (s c) -> p s c", s=4), func=AF.Copy),
                                reads=[tk], writes=["ztok"])
                        defer(post_z)
                    flush()
                    for sub in range(4):
                        for i in range(4):
                            P.op("dve", lambda v, sub=sub, i=i: v.bn_stats(out=st6[:, i, :], in_=ztok[:, sub, i * 512:(i + 1) * 512]),
                                 reads=["ztok"], writes=["st6"])
                        P.op("dve", lambda v: v.bn_aggr(out=mv[:], in_=st6[:].rearrange("p a b -> p (a b)")), reads=["st6"], writes=["mv"])
                        P.op("dve", lambda v: v.tensor_scalar(out=rs[:, 0:1], in0=mv[:, 1:2], scalar1=EPS, scalar2=None, op0=ALU.add),
                             reads=["mv"], writes=["rs"])
                        P.op("act", lambda a: a.activation(out=rs[:, 0:1], in_=rs[:, 0:1], func=AF.Sqrt), reads=["rs"], writes=["rs"])
                        P.op("dve", lambda v: v.reciprocal(out=rs[:, 0:1], in_=rs[:, 0:1]), reads=["rs"], writes=["rs"])
                        P.op("dve", lambda v: v.scalar_tensor_tensor(out=rs[:, 1:2], in0=mv[:, 0:1], scalar=-1.0, in1=rs[:, 0:1],
                                                                     op0=ALU.mult, op1=ALU.mult), reads=["mv", "rs"], writes=["rs"])
                        P.op("dve", lambda v, sub=sub: v.tensor_scalar(out=zf[:], in0=ztok[:, sub, :], scalar1=rs[:, 0:1], scalar2=rs[:, 1:2],
                                                                       op0=ALU.mult, op1=ALU.add), reads=["ztok", "rs"], writes=["zf"])
                        P.op("dve", lambda v: v.tensor_tensor(out=zf[:], in0=zf[:], in1=lng[:], op=ALU.mult), reads=["zf", "lng"], writes=["zf"])
                        P.op("dve", lambda v, sub=sub: v.tensor_tensor(out=zn[:, sub, :], in0=zf[:], in1=lnb[:], op=ALU.add),
                             reads=["zf", "lnb"], writes=["zn"])
                    for uc in range(16):
                        g = uc // 2
                        wt, wk = WS.load(w_in_v, OFF_U + uc * 128, 128, KD, wc_in)
                        pa, pk = next_acc()
                        proj(wt, wk, 128, KD, hT, hT_keys, pa, pk)
                        ub = gz[uc % 2]
                        gelu_from_psum(e3, pa, pk, ub[:], ("gz", uc % 2), (gx[uc % 2], gt[uc % 2]), uc % 2)
                        px, pxk = next_acc()
                        for sub in range(4):
                            cs = slice(sub * 128, (sub + 1) * 128)
                            P.op("pe", lambda pe, px=px, sub=sub, uc=uc, g=g, cs=cs: pe.matmul(
                                px[:, cs], lhsT=zn[:, sub, uc * 128:(uc + 1) * 128], rhs=wsT_b[:, g, :], start=True, stop=False),
                                reads=["zn", "wsT_b"], writes=[pxk], inc=False)
                            P.op("pe", lambda pe, px=px, g=g, cs=cs: pe.matmul(
                                px[:, cs], lhsT=ones_b_t[0:1, :], rhs=bhi[0:1, g * 128:(g + 1) * 128], start=False, stop=False),
                                reads=["onesb", "bhi"], writes=[pxk], inc=False)
                            P.op("pe", lambda pe, px=px, g=g, cs=cs: pe.matmul(
                                px[:, cs], lhsT=ones_b_t[0:1, :], rhs=blo[0:1, g * 128:(g + 1) * 128], start=False, stop=True),
                                reads=["onesb", "blo"], writes=[pxk], inc=(sub == 3))
                        P.op("dve", lambda v, px=px, ub=ub, uc=uc: v.tensor_tensor(out=suT[:, uc, :], in0=px[:], in1=ub[:], op=ALU.mult),
                             reads=[pxk, ("gz", uc % 2)], writes=[("suT", uc)])
                    P.barrier(pool=False)
                P.mark(f'B4_{tt}')
                with ExitStack() as e4:
                    mT = sb(e4, "mT", [128, KD, TT], BF16)
                    sa = sb(e4, "sa", [128, TT], F32)
                    sbb = sb(e4, "sbb", [128, TT], F32)
                    t1 = sb(e4, "m_t1", [128, TT], F32)
                    t2 = sb(e4, "m_t2", [128, TT], F32)
                    og_keys = [("ogT", k) for k in range(16)]
                    su_keys = [("suT", k) for k in range(16)]
                    for j in range(KD):
                        wt, wk = WS.load(wa_v, j * 128, 128, 16, wc_a)
                        pA, pAk = next_acc()
                        proj(wt, wk, 128, 16, ogT, og_keys, pA, pAk)
                        wt, wk = WS.load(w_in_v, OFF_GA + j * 128, 128, KD, wc_in)
                        pGA, pGAk = next_acc()
                        proj(wt, wk, 128, KD, hT, hT_keys, pGA, pGAk)
                        P.op("act", lambda a, pGA=pGA: a.activation(out=sa[:], in_=pGA[:], func=AF.Sigmoid), reads=[pGAk], writes=["sa"])
                        P.op("dve", lambda v, pA=pA: v.tensor_tensor(out=t1[:], in0=pA[:], in1=sa[:], op=ALU.mult), reads=[pAk, "sa"], writes=["m_t1"])
                        wt, wk = WS.load(wb_v, j * 128, 128, 16, wc_b)
                        pB, pBk = next_acc()
                        proj(wt, wk, 128, 16, suT, su_keys, pB, pBk)
                        wt, wk = WS.load(w_in_v, OFF_GB + j * 128, 128, KD, wc_in)
                        pGB, pGBk = next_acc()
                        proj(wt, wk, 128, KD, hT, hT_keys, pGB, pGBk)
                        P.op("act", lambda a, pGB=pGB: a.activation(out=sbb[:], in_=pGB[:], func=AF.Sigmoid), reads=[pGBk], writes=["sbb"])
                        P.op("dve", lambda v, pB=pB: v.tensor_tensor(out=t2[:], in0=pB[:], in1=sbb[:], op=ALU.mult), reads=[pBk, "sbb"], writes=["m_t2"])
                        P.op("dve", lambda v, j=j: v.tensor_tensor(out=mT[:, j, :], in0=t1[:], in1=t2[:], op=ALU.add),
                             reads=["m_t1", "m_t2"], writes=[("mT", j)])
                    P.mark(f'B5_{tt}')
                    mT_keys = [("mT", k) for k in range(KD)]
                    xj = [sb(e4, f"xj{i}", [128, TT], F32) for i in range(2)]
                    x1T = [sb(e4, f"x1T{i}", [128, TT], F32) for i in range(2)]
                    xst = [sb(e4, f"xst{i}", [128, 4, 128], F32) for i in range(2)]
                    for j in range(KD):
                        jb = j % 2
                        wt, wk = WS.load(wo_v, j * 128, 128, KD, wc_o)
                        pO, pOk = next_acc()
                        proj(wt, wk, 128, KD, mT, mT_keys, pO, pOk)
                        P.dma("sp", lambda q, j=j, jb=jb: q.dma_start(out=xj[jb][:], in_=xT_v[:, j, tt * TT:(tt + 1) * TT]),
                              writes=[("xj", jb)])
                        P.op("dve", lambda v, pO=pO, j=j, jb=jb: v.scalar_tensor_tensor(
                            out=x1T[jb][:], in0=pO[:], scalar=g1T[:, j:j + 1], in1=xj[jb][:], op0=ALU.mult, op1=ALU.add),
                            reads=[pOk, "modT", ("xj", jb)], writes=[("x1T", jb)])
                        def post_o(j=j, jb=jb, tt=tt):
                            P.op("pe", lambda pe, j=j, jb=jb: pe.matmul(pL[0:32, :], lhsT=wr[:, j, :], rhs=x1T[jb][:],
                                                                        start=(j == 0), stop=(j == KD - 1)),
                                 reads=["wr", ("x1T", jb)], writes=["pb_L"])
                            for sub in range(4):
                                P.op("pe", lambda pe, sub=sub, jb=jb: pe.transpose(
                                    out=pX[:, sub * 128:(sub + 1) * 128], in_=x1T[jb][:, sub * 128:(sub + 1) * 128], identity=ident_f),
                                    reads=[("x1T", jb), "cst"], writes=["pb_X"], inc=(sub == 3))
                            P.op("act", lambda a, jb=jb: a.activation(out=xst[jb][:], in_=pX[:].rearrange("p (s c) -> p s c", s=4), func=AF.Copy),
                                 reads=["pb_X"], writes=[("xst", jb)])
                            P.dma("sp", lambda q, j=j, jb=jb, tt=tt: q.dma_start(
                                out=x1_d[tt * TT:(tt + 1) * TT, j * 128:(j + 1) * 128].rearrange("(s p) c -> p s c", p=128),
                                in_=xst[jb][:]), reads=[("xst", jb)], writes=[("x1_d", tt, j)])
                        defer(post_o)
                    flush()
                    P.op("act", lambda a, tt=tt: a.activation(out=lgT[:, tt * TT:(tt + 1) * TT], in_=pL[0:32, :], func=AF.Copy),
                         reads=["pb_L"], writes=["lgT"])
                    P.barrier(pool=False)
            P.barrier()


        NSUB = TOK // 128
        NBLK = CAP // 128
        if STAGE >= 2:
          with ExitStack() as eM:
            slots = sb(eM, "slots", [128, NSUB, 4], mybir.dt.int32)
            bnd_reg = nc.gpsimd.to_reg(NE * CAP - 1)
            wk_all = sb(eM, "wk_all", [128, NSUB, 4], F32)
            P.mark('2a')
            with ExitStack() as e2:
                gm2b = sb(e2, "gm2b", [128, D], F32)
                sh2b = sb(e2, "sh2b", [128, D], F32)
                tA = sb(e2, "tA", [128, D], F32)
                tB = sb(e2, "tB", [128, D], F32)
                bc = lambda ap: ap.partition_broadcast(128)
                P.dma("sp", lambda q: q.dma_start(out=tA[:], in_=bc(mod_d[0, 4 * D:5 * D])), writes=["tA"])
                P.dma("sp", lambda q: q.dma_start(out=tB[:], in_=bc(I["b_ada_row"][0, 4 * D:5 * D])), writes=["tB"])
                P.op("dve", lambda v: v.tensor_tensor(out=tA[:], in0=tA[:], in1=tB[:], op=ALU.add), reads=["tA", "tB"], writes=["tA"])
                P.dma("sp", lambda q: q.dma_start(out=tB[:], in_=bc(I["norm_ffn_g"][0, :])), reads=["tB"], writes=["tB"])
                P.op("dve", lambda v: v.scalar_tensor_tensor(out=gm2b[:], in0=tA[:], scalar=1.0, in1=tB[:], op0=ALU.add, op1=ALU.mult),
                     reads=["tA", "tB"], writes=["gm2b"])
                P.dma("sp", lambda q: q.dma_start(out=tA[:], in_=bc(mod_d[0, 3 * D:4 * D])), reads=["tA"], writes=["tA"])
                P.dma("sp", lambda q: q.dma_start(out=tB[:], in_=bc(I["b_ada_row"][0, 3 * D:4 * D])), reads=["tB"], writes=["tB"])
                P.op("dve", lambda v: v.tensor_tensor(out=sh2b[:], in0=tA[:], in1=tB[:], op=ALU.add), reads=["tA", "tB"], writes=["sh2b"])
                wr2 = sb(e2, "wr2", [128, KD, NE], F32)
                brow = sb(e2, "brow", [1, NE], F32)
                crow = sb(e2, "crow", [1, NE], F32)
                constb = sb(e2, "constb", [128, NE], F32)
                ecap = sb(e2, "ecap", [128, NE], F32)
                macc = sb(e2, "macc", [128, NE], F32)
                pq = ps(e2, "pq", [128, 512], F32)
                pr = ps(e2, "pr", [128, 512], F32)
                P.dma("sp", lambda q: q.dma_start(out=wr2[:], in_=I["w_routerT"][:, :, :]), writes=["wr2"])
                P.dma("sp", lambda q: q.dma_start(out=brow[:], in_=I["b_router"][:, :]), writes=["brow"])
                for k in range(KD):
                    P.op("pe", lambda pe, k=k: pe.matmul(pq[0:1, 0:NE], lhsT=sh2T[:, k:k + 1], rhs=wr2[:, k, :],
                                                         start=(k == 0), stop=(k == KD - 1)), reads=["modT", "wr2"], writes=["pq"], inc=(k == KD - 1))
                P.op("dve", lambda v: v.tensor_tensor(out=crow[:], in0=pq[0:1, 0:NE], in1=brow[:], op=ALU.add), reads=["pq", "brow"], writes=["crow"])
                P.op("pe", lambda pe: pe.matmul(pq[:, 0:NE], lhsT=ones_f[0:1, :], rhs=crow[:], start=True, stop=True),
                     reads=["cst", "crow"], writes=["pq"])
                P.op("dve", lambda v: v.tensor_copy(out=constb[:], in_=pq[:, 0:NE]), reads=["pq"], writes=["constb"])
                P.op("dve", lambda v: v.tensor_scalar(out=ecap[:], in0=cst[:, 768:800], scalar1=float(CAP), scalar2=None, op0=ALU.mult),
                     reads=["cst"], writes=["ecap"])
                P.op("dve", lambda v: v.memset(macc[:], 0.0), writes=["macc"])
                x1t = [sb(e2, f"x1t{i}", [128, D], F32) for i in range(2)]
                h2t = [sb(e2, f"h2t{i}", [128, D], BF16) for i in range(2)]
                sm = sb(e2, "sm", [128, 8], F32)
                lg = sb(e2, "lg", [128, NE], F32)
                m8 = sb(e2, "m8", [128, 8], F32)
                msk = sb(e2, "msk", [128, NE], F32)
                pe_ = sb(e2, "pe_", [128, NE], F32)
                wts = sb(e2, "wts", [128, NE], F32)
                slotd = sb(e2, "slotd", [128, NE], F32)
                oh = sb(e2, "oh", [128, NE], F32)
                jk = sb(e2, "jk", [128, NE], F32)
                slf = sb(e2, "slf", [128, 4], F32)
                for s_ in range(NSUB):
                    b = s_ % 2
                    xk, hk = ("x1t", b), ("h2t", b)
                    P.dma("sp", lambda q, b=b, s_=s_: q.dma_start(out=x1t[b][:], in_=x1_d[s_ * 128:(s_ + 1) * 128, :]), writes=[xk])
                    P.op("act", lambda a, b=b: a.activation(out=tA[:], in_=x1t[b][:], func=AF.Square, accum_out=sm[:, 0:1]),
                         reads=[xk], writes=["tA", "sm"])
                    P.op("dve", lambda v: v.tensor_scalar(out=sm[:, 1:2], in0=sm[:, 0:1], scalar1=1.0 / D, scalar2=EPS, op0=ALU.mult, op1=ALU.add),
                         reads=["sm"], writes=["sm"])
                    P.op("act", lambda a: a.activation(out=sm[:, 1:2], in_=sm[:, 1:2], func=AF.Sqrt), reads=["sm"], writes=["sm"])
                    P.op("dve", lambda v: v.reciprocal(out=sm[:, 2:3], in_=sm[:, 1:2]), reads=["sm"], writes=["sm"])
                    P.op("dve", lambda v, b=b: v.scalar_tensor_tensor(out=tB[:], in0=x1t[b][:], scalar=sm[:, 2:3], in1=gm2b[:],
                                                                      op0=ALU.mult, op1=ALU.mult), reads=[xk, "sm", "gm2b"], writes=["tB"])
                    P.op("dve", lambda v, b=b: v.tensor_tensor(out=h2t[b][:], in0=tB[:], in1=sh2b[:], op=ALU.add),
                         reads=["tB", "sh2b"], writes=[hk])
                    P.op("pe", lambda pe, s_=s_: pe.transpose(out=pr[:, 0:NE], in_=lgT[0:32, s_ * 128:(s_ + 1) * 128], identity=ident_f[0:32, 0:32]),
                         reads=["lgT", "cst"], writes=["pr"])
                    P.op("dve", lambda v: v.scalar_tensor_tensor(out=lg[:], in0=pr[:, 0:NE], scalar=sm[:, 2:3], in1=constb[:],
                                                                 op0=ALU.mult, op1=ALU.add), reads=["pr", "sm", "constb"], writes=["lg"])
                    P.op("dve", lambda v: v.max(out=m8[:], in_=lg[:]), reads=["lg"], writes=["m8"])
                    P.op("dve", lambda v: v.tensor_scalar(out=msk[:], in0=lg[:], scalar1=m8[:, 3:4], scalar2=None, op0=ALU.is_ge),
                         reads=["lg", "m8"], writes=["msk"])
                    P.op("dve", lambda v: v.tensor_scalar(out=sm[:, 3:4], in0=m8[:, 0:1], scalar1=-1.0, scalar2=None, op0=ALU.mult),
                         reads=["m8"], writes=["sm"])
                    P.op("act", lambda a: a.activation(out=pe_[:], in_=lg[:], func=AF.Exp, bias=sm[:, 3:4], scale=1.0),
                         reads=["lg", "sm"], writes=["pe_"])
                    P.op("dve", lambda v: v.tensor_tensor(out=wts[:], in0=pe_[:], in1=msk[:], op=ALU.mult),
                         reads=["pe_", "msk"], writes=["wts"])
                    P.op("dve", lambda v: v.tensor_reduce(out=sm[:, 4:5], in_=wts[:], axis=mybir.AxisListType.X, op=ALU.add),
                         reads=["wts"], writes=["sm"])
                    P.op("dve", lambda v: v.reciprocal(out=sm[:, 5:6], in_=sm[:, 4:5]), reads=["sm"], writes=["sm"])
                    P.op("dve", lambda v: v.tensor_scalar(out=wts[:], in0=wts[:], scalar1=sm[:, 5:6], scalar2=None, op0=ALU.mult),
                         reads=["wts", "sm"], writes=["wts"])
                    P.op("pe", lambda pe: pe.matmul(pq[:, 0:NE], lhsT=cst[:, 640:768], rhs=msk[:], start=True, stop=False),
                         reads=["cst", "msk"], writes=["pq"], inc=False)
                    P.op("pe", lambda pe: pe.matmul(pq[:, 0:NE], lhsT=ones_f, rhs=macc[:], start=False, stop=True),
                         reads=["cst", "macc"], writes=["pq"])
                    P.op("dve", lambda v: v.tensor_tensor(out=macc[:], in0=macc[:], in1=msk[:], op=ALU.add), reads=["macc", "msk"], writes=["macc"])
                    P.op("dve", lambda v: v.tensor_scalar(out=jk[:], in0=pq[:, 0:NE], scalar1=float(CAP), scalar2=None, op0=ALU.is_lt),
                         reads=["pq"], writes=["jk"])
                    P.op("dve", lambda v: v.tensor_tensor(out=jk[:], in0=jk[:], in1=msk[:], op=ALU.mult), reads=["jk", "msk"], writes=["jk"])
                    P.op("dve", lambda v: v.tensor_tensor(out=slotd[:], in0=pq[:, 0:NE], in1=ecap[:], op=ALU.add), reads=["pq", "ecap"], writes=["slotd"])
                    P.op("dve", lambda v: v.scalar_tensor_tensor(out=slotd[:], in0=slotd[:], scalar=-float(4 * NE * CAP), in1=jk[:],
                                                                 op0=ALU.add, op1=ALU.mult), reads=["slotd", "jk"], writes=["slotd"])
                    P.op("dve", lambda v: v.tensor_scalar(out=slotd[:], in0=slotd[:], scalar1=float(4 * NE * CAP), scalar2=None, op0=ALU.add),
                         reads=["slotd"], writes=["slotd"])
                    for k in range(4):
                        P.op("dve", lambda v, k=k: v.tensor_scalar(out=oh[:], in0=lg[:], scalar1=m8[:, k:k + 1], scalar2=None, op0=ALU.is_equal),
                             reads=["lg", "m8"], writes=["oh"])
                        P.op("dve", lambda v: v.tensor_tensor(out=jk[:], in0=oh[:], in1=slotd[:], op=ALU.mult),
                             reads=["oh", "slotd"], writes=["jk"])
                        P.op("dve", lambda v, k=k: v.tensor_reduce(out=slf[:, k:k + 1], in_=jk[:], axis=mybir.AxisListType.X, op=ALU.add),
                             reads=["jk"], writes=["slf"])
                        P.op("dve", lambda v: v.tensor_tensor(out=jk[:], in0=oh[:], in1=wts[:], op=ALU.mult),
                             reads=["oh", "wts"], writes=["jk"])
                        P.op("dve", lambda v, k=k, s_=s_: v.tensor_reduce(out=wk_all[:, s_, k:k + 1], in_=jk[:], axis=mybir.AxisListType.X, op=ALU.add),
                             reads=["jk"], writes=["wk_all"])
                    P.op("dve", lambda v, s_=s_: v.tensor_copy(out=slots[:, s_, :], in_=slf[:]), reads=["slf"], writes=["slots"])
                    for k in range(4):
                        P.dma("pool", lambda q, b=b, s_=s_, k=k: q.indirect_dma_start(
                            out=Xd[:, :], out_offset=bass.IndirectOffsetOnAxis(ap=slots[:, s_, k:k + 1].bitcast(U32), axis=0),
                            in_=h2t[b][:], in_offset=None, bounds_check=bnd_reg, oob_is_err=False),
                            reads=[hk, "slots"], writes=[("Xd", s_, k)])
                P.barrier()
            P.mark('2c')
            with ExitStack() as e3:
                XeTs = [sb(e3, f"XeT{i}", [128, KD, CAP], BF16) for i in range(2)]
                XeTk = [[("XeT", i, k) for k in range(KD)] for i in range(2)]
                xtok = [sb(e3, f"xtok{i}", [128, D], BF16) for i in range(2)]
                WG = WStream(e3, "wsG", 4, KD)
                wdb = [sb(e3, f"wdb{i}", [128, 8, 512], BF16) for i in range(2)]
                actT = sb(e3, "actT", [128, 8, CAP], BF16)
                bgT = sb(e3, "bgT", [128, NE, 8], F32)
                buT = sb(e3, "buT", [128, NE, 8], F32)
                bdf = sb(e3, "bdf", [1, D], F32)
                bdb = sb(e3, "bdb", [1, D], BF16)
                xg = sb(e3, "xg", [128, CAP], F32)
                sgm = sb(e3, "sgm", [128, CAP], F32)
                xl = sb(e3, "xl", [128, CAP], F32)
                yst = [sb(e3, f"yst{i}", [128, 512], F32) for i in range(3)]
                P.dma("sp", lambda q: q.dma_start(out=bgT[:], in_=I["b_gateT"][:, :, :]), writes=["bgT"])
                P.dma("sp", lambda q: q.dma_start(out=buT[:], in_=I["b_upT"][:, :, :]), writes=["buT"])
                pTe = [ps(e3, f"pTe{i}", [128, 1024], BF16) for i in range(2)]
                pg_ = [ps(e3, f"pg{i}", [128, 512], F32) for i in range(2)]
                pu_ = [ps(e3, f"pu{i}", [128, 512], F32) for i in range(2)]
                po_ = [ps(e3, f"po{i}", [128, 512], F32) for i in range(2)]
                nx = [0]
                ny = 0
                ndw = 0

                def xet_gen(e_):
                    XeT = XeTs[e_ % 2]
                    for blk in range(NBLK):
                        xb_ = xtok[nx[0] % 2]
                        xbk = ("xtok", nx[0] % 2)
                        nx[0] += 1
                        r0 = e_ * CAP + blk * 128
                        P.dma("sp", lambda q, xb_=xb_, r0=r0: q.dma_start(out=xb_[:], in_=Xd[r0:r0 + 128, :]), writes=[xbk])
                        for k4 in range(8):
                            tp = pTe[k4 % 2]
                            tk = ("pTe", k4 % 2)
                            for kk in range(4):
                                k = k4 * 4 + kk
                                P.op("pe", lambda pe, tp=tp, kk=kk, k=k, xb_=xb_: pe.transpose(
                                    out=tp[:, kk * 128:(kk + 1) * 128], in_=xb_[:, k * 128:(k + 1) * 128], identity=ident_b),
                                    reads=[xbk, "identb"], writes=[tk], inc=(kk == 3))
                            wkeys = [("XeT", e_ % 2, k4 * 4 + kk) for kk in range(4)]
                            if k4 % 2 == 0:
                                P.op("act", lambda a, tp=tp, k4=k4, blk=blk, XeT=XeT: a.activation(
                                    out=XeT[:, k4 * 4:(k4 + 1) * 4, blk * 128:(blk + 1) * 128],
                                    in_=tp[:, 0:512].rearrange("p (s c) -> p s c", s=4), func=AF.Copy), reads=[tk], writes=wkeys)
                            else:
                                P.op("dve", lambda v, tp=tp, k4=k4, blk=blk, XeT=XeT: v.tensor_copy(
                                    out=XeT[:, k4 * 4:(k4 + 1) * 4, blk * 128:(blk + 1) * 128],
                                    in_=tp[:, 0:512].rearrange("p (s c) -> p s c", s=4)), reads=[tk], writes=wkeys)
                            yield

                step(xet_gen(0), 1000)
                for e in range(NE):
                    XeT = XeTs[e % 2]
                    XeT_keys = XeTk[e % 2]
                    bg = xet_gen(e + 1) if e + 1 < NE else None
                    wg_v = I["w_exp_gate"][e].rearrange("(k p) c -> p k c", p=128)
                    wu_v = I["w_exp_up"][e].rearrange("(k p) c -> p k c", p=128)
                    wd_v = I["w_exp_down"][e].rearrange("(k p) c -> p k c", p=128)
                    P.dma("sp", lambda q, e=e: q.dma_start(out=bdf[:], in_=I["b_exp_down"][e:e + 1, :]), writes=["bdf"])
                    P.op("dve", lambda v: v.tensor_copy(out=bdb[:], in_=bdf[:]), reads=["bdf"], writes=["bdb"])
                    for fc in range(8):
                        pg, pgk = pg_[fc % 2], ("pg", fc % 2)
                        pu, puk = pu_[fc % 2], ("pu", fc % 2)
                        wt, wk = WG.load(wg_v, fc * 128, 128, KD)
                        proj(wt, wk, 128, KD, XeT, XeT_keys, pg, pgk)
                        wt, wk = WG.load(wu_v, fc * 128, 128, KD)
                        proj(wt, wk, 128, KD, XeT, XeT_keys, pu, puk)
                        step(bg, 4)
                        P.op("dve", lambda v, pg=pg, e=e, fc=fc: v.tensor_scalar(out=xg[:], in0=pg[:], scalar1=bgT[:, e, fc:fc + 1], scalar2=7.0,
                                                                                 op0=ALU.add, op1=ALU.min), reads=[pgk, "bgT"], writes=["xg"])
                        P.op("act", lambda a: a.activation(out=sgm[:], in_=xg[:], func=AF.Sigmoid, scale=1.702), reads=["xg"], writes=["sgm"])
                        P.op("dve", lambda v, pu=pu, e=e, fc=fc: v.tensor_scalar(out=xl[:], in0=pu[:], scalar1=buT[:, e, fc:fc + 1], scalar2=7.0,
                                                                                 op0=ALU.add, op1=ALU.min), reads=[puk, "buT"], writes=["xl"])
                        P.op("dve", lambda v: v.tensor_scalar(out=xl[:], in0=xl[:], scalar1=-7.0, scalar2=1.0, op0=ALU.max, op1=ALU.add),
                             reads=["xl"], writes=["xl"])
                        P.op("dve", lambda v: v.tensor_tensor(out=xg[:], in0=xg[:], in1=sgm[:], op=ALU.mult), reads=["xg", "sgm"], writes=["xg"])
                        P.op("dve", lambda v, fc=fc: v.tensor_tensor(out=actT[:, fc, :], in0=xg[:], in1=xl[:], op=ALU.mult),
                             reads=["xg", "xl"], writes=[("actT", fc)])
                    step(bg, 1000)
                    for db in range(8):
                        wd = wdb[ndw % 2]
                        wdk = ("wdb", ndw % 2)
                        ndw += 1
                        P.dma("pool", lambda q, wd=wd, db=db, wd_v=wd_v: q.dma_start(out=wd[:], in_=wd_v[:, :, db * 512:(db + 1) * 512]), writes=[wdk])
                        for blk in range(NBLK):
                            po, pok = po_[blk % 2], ("po", blk % 2)
                            for fk in range(8):
                                P.op("pe", lambda pe, po=po, fk=fk, blk=blk, wd=wd: pe.matmul(
                                    po[:], lhsT=actT[:, fk, blk * 128:(blk + 1) * 128], rhs=wd[:, fk, :], start=(fk == 0), stop=False),
                                    reads=[("actT", fk), wdk], writes=[pok], inc=False)
                            P.op("pe", lambda pe, po=po, db=db: pe.matmul(po[:], lhsT=ones_b_t[0:1, :], rhs=bdb[0:1, db * 512:(db + 1) * 512],
                                                                          start=False, stop=True), reads=["onesb", "bdb"], writes=[pok])
                            yb = yst[ny % 3]
                            ybk = ("yst", ny % 3)
                            if ny % 2 == 0:
                                P.op("act", lambda a, yb=yb, po=po: a.activation(out=yb[:], in_=po[:], func=AF.Copy), reads=[pok], writes=[ybk])
                            else:
                                P.op("dve", lambda v, yb=yb, po=po: v.tensor_copy(out=yb[:], in_=po[:]), reads=[pok], writes=[ybk])
                            ny += 1
                            r0 = e * CAP + blk * 128
                            P.dma("sp", lambda q, yb=yb, r0=r0, db=db: q.dma_start(out=Yd[r0:r0 + 128, db * 512:(db + 1) * 512], in_=yb[:]),
                                  reads=[ybk], writes=[("Yd", e, blk, db)])
                P.barrier()
            P.mark('2d')
            with ExitStack() as e4:
                g2b = sb(e4, "g2b", [128, D], F32)
                gfb = sb(e4, "gfb", [128, D], F32)
                tA = sb(e4, "tA2", [128, D], F32)
                bc = lambda ap: ap.partition_broadcast(128)
                P.dma("sp", lambda q: q.dma_start(out=g2b[:], in_=bc(mod_d[0, 5 * D:6 * D])), writes=["g2b"])
                P.dma("sp", lambda q: q.dma_start(out=tA[:], in_=bc(I["b_ada_row"][0, 5 * D:6 * D])), writes=["tA2"])
                P.op("dve", lambda v: v.tensor_tensor(out=g2b[:], in0=g2b[:], in1=tA[:], op=ALU.add), reads=["g2b", "tA2"], writes=["g2b"])
                P.dma("sp", lambda q: q.dma_start(out=gfb[:], in_=bc(I["norm_final_g"][0, :])), writes=["gfb"])
                x1t = [sb(e4, f"x1u{i}", [128, D], F32) for i in range(2)]
                rk = [sb(e4, f"rk{i}", [128, D], F32) for i in range(3)]
                acc = sb(e4, "acc", [128, D], F32)
                sm = sb(e4, "sm2", [128, 4], F32)
                nr = 0
                for s_ in range(NSUB):
                    b = s_ % 2
                    xk = ("x1u", b)
                    P.dma("sp", lambda q, b=b, s_=s_: q.dma_start(out=x1t[b][:], in_=x1_d[s_ * 128:(s_ + 1) * 128, :]), writes=[xk])
                    for k in range(4):
                        r = rk[nr % 3]
                        rkk = ("rk", nr % 3)
                        nr += 1
                        P.dma("pool", lambda q, r=r, s_=s_, k=k: q.indirect_dma_start(
                            out=r[:], out_offset=None, in_=Yd[:, :],
                            in_offset=bass.IndirectOffsetOnAxis(ap=slots[:, s_, k:k + 1].bitcast(U32), axis=0),
                            bounds_check=bnd_reg, oob_is_err=False), reads=["slots"], writes=[rkk])
                        if k == 0:
                            P.op("dve", lambda v, r=r, s_=s_, k=k: v.tensor_scalar(out=acc[:], in0=r[:], scalar1=wk_all[:, s_, k:k + 1], scalar2=None,
                                                                                   op0=ALU.mult), reads=[rkk, "wk_all"], writes=["acc"])
                        else:
                            P.op("dve", lambda v, r=r, s_=s_, k=k: v.scalar_tensor_tensor(out=acc[:], in0=r[:], scalar=wk_all[:, s_, k:k + 1], in1=acc[:],
                                                                                          op0=ALU.mult, op1=ALU.add), reads=[rkk, "wk_all", "acc"], writes=["acc"])
                    P.op("dve", lambda v: v.tensor_tensor(out=acc[:], in0=acc[:], in1=g2b[:], op=ALU.mult), reads=["acc", "g2b"], writes=["acc"])
                    P.op("dve", lambda v, b=b: v.tensor_tensor(out=acc[:], in0=acc[:], in1=x1t[b][:], op=ALU.add), reads=["acc", xk], writes=["acc"])
                    P.op("act", lambda a: a.activation(out=tA[:], in_=acc[:], func=AF.Square, accum_out=sm[:, 0:1]), reads=["acc"], writes=["tA2", "sm2"])
                    P.op("dve", lambda v: v.tensor_scalar(out=sm[:, 1:2], in0=sm[:, 0:1], scalar1=1.0 / D, scalar2=EPS, op0=ALU.mult, op1=ALU.add),
                         reads=["sm2"], writes=["sm2"])
                    P.op("act", lambda a: a.activation(out=sm[:, 1:2], in_=sm[:, 1:2], func=AF.Sqrt), reads=["sm2"], writes=["sm2"])
                    P.op("dve", lambda v: v.reciprocal(out=sm[:, 2:3], in_=sm[:, 1:2]), reads=["sm2"], writes=["sm2"])
                    P.op("dve", lambda v, b=b: v.scalar_tensor_tensor(out=x1t[b][:], in0=acc[:], scalar=sm[:, 2:3], in1=gfb[:], op0=ALU.mult, op1=ALU.mult),
                         reads=["acc", "sm2", "gfb"], writes=[xk])
                    P.dma("sp", lambda q, b=b, s_=s_: q.dma_start(out=out[s_ * 128:(s_ + 1) * 128, :], in_=x1t[b][:]), reads=[xk], writes=[("out", s_)])
                P.barrier()

        if STAGE < 2:
         with ExitStack() as eZ:
             tb = [sb(eZ, f"tb{i}", [128, D], F32) for i in range(2)]
             for s_ in range(TOK // 128):
                 b = s_ % 2
                 P.dma("sp", lambda q, b=b, s_=s_: q.dma_start(out=tb[b][:], in_=x1_d[s_ * 128:(s_ + 1) * 128, :]),
                       writes=[("tb", b)])
                 P.dma("sp", lambda q, b=b, s_=s_: q.dma_start(out=out[s_ * 128:(s_ + 1) * 128, :], in_=tb[b][:]),
                       reads=[("tb", b)], writes=[("out", s_)])
             P.barrier()
    build_nc.marks = P.marks
    return nc


def make_consts():
    c = np.zeros((128, 7 * 128), np.float32)
    j = np.arange(128)[:, None]
    i = np.arange(128)[None, :]
    c[:, 0:128] = np.eye(128, dtype=np.float32)
    c[:, 128:256] = 1.0
    c[:, 256:384] = np.where(j <= i, -1.0 / 16.0, 0.0)
    c[:, 384:512] = np.where(j > i, -1.0 / 16.0, 0.0)
    c[:, 512:640] = np.where(j <= i, 1.0, 0.0)
    c[:, 640:768] = np.where(j < i, 1.0, 0.0)
    c[:, 768:800] = np.arange(32)[None, :]
    return c


def fm(v):
    v = np.asarray(v)
    return np.ascontiguousarray(v.reshape(-1, 128).T)


def prep_inputs(x, c, w_ada, b_ada, norm_mix_g, w_in, w_alpha_up, b_alpha, gla_norm_g, sgu_ln_g, sgu_ln_b,
                w_spatial, b_spatial, w_branch_a, w_branch_b, w_out, norm_ffn_g, w_router, b_router,
                w_exp_gate, b_exp_gate, w_exp_up, b_exp_up, w_exp_down, b_exp_down, norm_final_g):
    f = lambda a: np.ascontiguousarray(np.asarray(a, dtype=np.float32))
    shared = {}
    shared["consts"] = make_consts()
    shared["w_ada"] = f(w_ada[0])
    shared["b_adaT"] = fm(f(b_ada[0]))
    shared["b_ada_row"] = f(b_ada[0]).reshape(1, -1)
    shared["gmixT"] = fm(f(norm_mix_g[0]))
    shared["w_in"] = f(w_in[0])
    wup = np.zeros((32, 1024), np.float32)
    wup[0:16] = f(w_alpha_up[0])
    wup[16] = f(b_alpha[0])
    shared["w_up_ext"] = wup
    shared["gla_norm_g"] = f(gla_norm_g[0]).reshape(1, -1)
    shared["sgu_ln_g"] = f(sgu_ln_g[0]).reshape(1, -1)
    shared["sgu_ln_b"] = f(sgu_ln_b[0]).reshape(1, -1)
    shared["wsT"] = np.ascontiguousarray(f(w_spatial[0]).transpose(2, 0, 1))
    shared["b_spatial"] = f(b_spatial[0]).reshape(1, -1)
    shared["w_branch_a"] = f(w_branch_a[0])
    shared["w_branch_b"] = f(w_branch_b[0])
    shared["w_out"] = f(w_out[0])
    shared["gffnT"] = fm(f(norm_ffn_g[0]))
    shared["norm_ffn_g"] = f(norm_ffn_g[0]).reshape(1, -1)
    shared["w_routerT"] = np.ascontiguousarray(f(w_router[0]).reshape(KD, 128, NE).transpose(1, 0, 2))
    shared["b_router"] = f(b_router[0]).reshape(1, -1)
    shared["w_exp_gate"] = f(w_exp_gate[0])
    shared["b_gateT"] = np.ascontiguousarray(f(b_exp_gate[0]).reshape(NE, 8, 128).transpose(2, 0, 1))
    shared["w_exp_up"] = f(w_exp_up[0])
    shared["b_upT"] = np.ascontiguousarray(f(b_exp_up[0]).reshape(NE, 8, 128).transpose(2, 0, 1))
    shared["w_exp_down"] = f(w_exp_down[0])
    shared["b_exp_down"] = f(b_exp_down[0])
    shared["norm_final_g"] = f(norm_final_g).reshape(1, -1)
    x = np.asarray(x, dtype=np.float32)
    c = np.asarray(c, dtype=np.float32)
    in_maps = []
    for ci in range(8):
        b, s = ci // 4, ci % 4
        xs = x[b, s * TOK:(s + 1) * TOK]
        m = dict(shared)
        m["x"] = np.ascontiguousarray(xs)
        m["xT"] = np.ascontiguousarray(xs.T)
        m["cT"] = fm(c[b])
        xp = np.zeros((D, 3 * TOK), np.float32)
        pm = np.zeros((128, 4), np.float32)
        for j in range(3):
            sj = s - 3 + j
            if sj >= 0:
                xp[:, j * TOK:(j + 1) * TOK] = x[b, sj * TOK:(sj + 1) * TOK].T
                pm[:, j] = 1.0
        m["xpT"] = xp
        m["pmask"] = pm
        in_maps.append(m)
    return in_maps


def kernel(**inputs):
    in_maps = prep_inputs(**inputs)
    nc = build_nc()
    names = set()
    for alloc in nc.allocations:
        if isinstance(alloc, mybir.MemoryLocationSet) and alloc.kind == "ExternalInput":
            names.add(alloc.memorylocations[0].name)
    in_maps = [{k: v for k, v in m.items() if k in names} for m in in_maps]
    res = run_bass_kernel_spmd(nc, in_maps, core_ids=list(range(8)))
    out = np.zeros((2, 4 * TOK, D), np.float32)
    for ci in range(8):
        b, s = ci // 4, ci % 4
        out[b, s * TOK:(s + 1) * TOK] = res.results[ci]["out"]
    return out
```
